# Optimizing a Trainium2 kernel written in Bass

```python
import math
import jax, jax.numpy as jnp
from jax import lax
import numpy as np

D_MODEL = 1024
BATCH = 4
SEQ = 8192
DEPTH = 1

MOBA_HEADS = 8
MOBA_HEAD_DIM = 64
MOBA_BLOCK = 256
MOBA_TOPK = 3
MOBA_QCHUNK = 64
SWA_Q_HEADS = 8
SWA_KV_HEADS = 2
SWA_HEAD_DIM = 64
SWA_WINDOW = 128
SWA_BLOCK = 128
MEM_HEADS = 4
MEM_HEAD_DIM = 128
MEM_LEN = 256
NUM_BUCKETS = 32
MAX_EXACT = NUM_BUCKETS // 2
REL_MAX_DISTANCE = 128
N_SELF_HEADS = MOBA_HEADS + SWA_Q_HEADS
FFN_HIDDEN = 2816
CONV_WIDTH = 3
RMS_EPS = 1e-6
MOBA_W = MOBA_HEADS * MOBA_HEAD_DIM
SWA_QW = SWA_Q_HEADS * SWA_HEAD_DIM
SWA_KVW = SWA_KV_HEADS * SWA_HEAD_DIM
MEM_W = MEM_HEADS * MEM_HEAD_DIM
IN_SPLITS = (MOBA_W, MOBA_W, MOBA_W, SWA_QW, SWA_KVW, SWA_KVW, MEM_W, D_MODEL, D_MODEL, D_MODEL)
IN_WIDTH = 3 * MOBA_W + SWA_QW + 2 * SWA_KVW + MEM_W + 3 * D_MODEL

kernel_name = "hybrid_moba_swa_mem_convffn"


def rmsnorm(x, g):
    xf = x.astype(jnp.float32)
    r = lax.rsqrt(jnp.mean(xf * xf, axis=-1, keepdims=True) + RMS_EPS)
    return (xf * r * g.astype(jnp.float32)).astype(x.dtype)


def t5_bucket(dist):
    n = jnp.maximum(dist, 0)
    nf = jnp.maximum(n, 1).astype(jnp.float32)
    large = MAX_EXACT + (jnp.log(nf / MAX_EXACT) / math.log(REL_MAX_DISTANCE / MAX_EXACT)
                         * (NUM_BUCKETS - MAX_EXACT)).astype(jnp.int32)
    large = jnp.minimum(large, NUM_BUCKETS - 1)
    return jnp.where(n < MAX_EXACT, n, large)


def moba_attention(q, k, v, bias_table):
    B_, S_, H, dh = q.shape
    Sp = -(-S_ // MOBA_BLOCK) * MOBA_BLOCK
    pad = Sp - S_
    if pad:
        padw = ((0, 0), (0, pad), (0, 0), (0, 0))
        q, k, v = jnp.pad(q, padw), jnp.pad(k, padw), jnp.pad(v, padw)
    NB = Sp // MOBA_BLOCK
    scale = 1.0 / math.sqrt(dh)
    qh = q.transpose(0, 2, 1, 3)
    kb = k.reshape(B_, NB, MOBA_BLOCK, H, dh).transpose(0, 3, 1, 2, 4)
    vb = v.reshape(B_, NB, MOBA_BLOCK, H, dh).transpose(0, 3, 1, 2, 4)
    kmean = jnp.mean(kb.astype(jnp.float32), axis=3)
    gate = jnp.einsum('bhsd,bhnd->bhsn', qh.astype(jnp.float32), kmean)
    qblk = jnp.arange(Sp) // MOBA_BLOCK
    past = jnp.arange(NB)[None, :] < qblk[:, None]
    gate = jnp.where(past, gate, -jnp.inf)
    k_sel = min(MOBA_TOPK, NB)
    _, sel = lax.top_k(gate, k_sel)
    sel_valid = jnp.arange(k_sel)[None, :] < qblk[:, None]
    bi = jnp.arange(B_)[:, None, None, None]
    hi = jnp.arange(H)[None, :, None, None]
    hb = jnp.arange(H)[None, :, None, None, None]
    kpos_in = jnp.arange(MOBA_BLOCK)

    def chunk(i):
        q0 = i * MOBA_QCHUNK
        qc = lax.dynamic_slice_in_dim(qh, q0, MOBA_QCHUNK, axis=2)
        selc = lax.dynamic_slice_in_dim(sel, q0, MOBA_QCHUNK, axis=2)
        validc = lax.dynamic_slice_in_dim(sel_valid, q0, MOBA_QCHUNK, axis=0)
        c = q0 // MOBA_BLOCK
        kg = kb[bi, hi, selc]
        vg = vb[bi, hi, selc]
        k_own = lax.dynamic_index_in_dim(kb, c, axis=2, keepdims=False)
        v_own = lax.dynamic_index_in_dim(vb, c, axis=2, keepdims=False)
        qpos = q0 + jnp.arange(MOBA_QCHUNK)
        kpos_sel = selc[..., None] * MOBA_BLOCK + kpos_in
        s_sel = (jnp.einsum('bhqd,bhqnkd->bhqnk', qc, kg).astype(jnp.float32) * scale
                 + bias_table[hb, t5_bucket(qpos[:, None, None] - kpos_sel)].astype(jnp.float32))
        s_sel = jnp.where(validc[:, :, None], s_sel, -jnp.inf)
        dist_own = qpos[:, None] - (c * MOBA_BLOCK + kpos_in)[None, :]
        s_own = (jnp.einsum('bhqd,bhkd->bhqk', qc, k_own).astype(jnp.float32) * scale
                 + bias_table[:, t5_bucket(dist_own)].astype(jnp.float32))
        s_own = jnp.where(dist_own >= 0, s_own, -jnp.inf)
        s_all = jnp.concatenate(
            [s_sel.reshape(B_, H, MOBA_QCHUNK, k_sel * MOBA_BLOCK), s_own], axis=-1)
        p = jax.nn.softmax(s_all, axis=-1).astype(v.dtype)
        p_sel = p[..., :k_sel * MOBA_BLOCK].reshape(B_, H, MOBA_QCHUNK, k_sel, MOBA_BLOCK)
        p_own = p[..., k_sel * MOBA_BLOCK:]
        return (jnp.einsum('bhqnk,bhqnkd->bhqd', p_sel, vg)
                + jnp.einsum('bhqk,bhkd->bhqd', p_own, v_own))

    outs = lax.map(chunk, jnp.arange(Sp // MOBA_QCHUNK))
    out = outs.transpose(1, 0, 3, 2, 4).reshape(B_, Sp, H, dh)
    return out[:, :S_]


def swa_attention(q, k, v, sinks, bias_table):
    B_, S_, HQ, dh = q.shape
    HKV = k.shape[2]
    G = HQ // HKV
    QB = SWA_BLOCK
    nblk = S_ // QB
    scale = 1.0 / math.sqrt(dh)
    qb = q.reshape(B_, nblk, QB, HKV, G, dh)
    kb = k.reshape(B_, nblk, QB, HKV, dh)
    vb = v.reshape(B_, nblk, QB, HKV, dh)
    padw = ((0, 0), (1, 0), (0, 0), (0, 0), (0, 0))
    kband = jnp.concatenate([jnp.pad(kb, padw)[:, :-1], kb], axis=2)
    vband = jnp.concatenate([jnp.pad(vb, padw)[:, :-1], vb], axis=2)
    s = jnp.einsum('bnqhgd,bnkhd->bhgnqk', qb, kband).astype(jnp.float32) * scale
    dist = QB + jnp.arange(QB)[:, None] - jnp.arange(2 * QB)[None, :]
    bias = bias_table[:, t5_bucket(dist)].astype(jnp.float32).reshape(HKV, G, 1, QB, 2 * QB)
    s = s + bias
    kpos = (jnp.arange(nblk)[:, None] - 1) * QB + jnp.arange(2 * QB)[None, :]
    allowed = ((dist >= 0) & (dist < SWA_WINDOW))[None] & (kpos >= 0)[:, None, :]
    s = jnp.where(allowed, s, -jnp.inf)
    sink = sinks.astype(jnp.float32).reshape(HKV, G, 1, 1, 1)
    m = jnp.maximum(jnp.max(s, axis=-1, keepdims=True), sink)
    p = jnp.exp(s - m)
    denom = jnp.sum(p, axis=-1, keepdims=True) + jnp.exp(sink - m)
    p = (p / denom).astype(v.dtype)
    o = jnp.einsum('bhgnqk,bnkhd->bnqhgd', p, vband)
    return o.reshape(B_, S_, HQ * dh)


def memory_attention(q, km, vm):
    B_, S_, Hm, dm = q.shape
    s = jnp.einsum('bshd,bmhd->bhsm', q, km).astype(jnp.float32) * (1.0 / math.sqrt(dm))
    p = jax.nn.softmax(s, axis=-1).astype(vm.dtype)
    return jnp.einsum('bhsm,bmhd->bshd', p, vm).reshape(B_, S_, Hm * dm)


def causal_depthwise_conv(u, w, b):
    S_ = u.shape[1]
    up = jnp.pad(u, ((0, 0), (CONV_WIDTH - 1, 0), (0, 0)))
    out = b.astype(u.dtype)
    for t in range(CONV_WIDTH):
        out = out + w[t].astype(u.dtype) * up[:, t:t + S_]
    return out


def _normal(k, shape, scale):
    return jax.random.normal(k, shape, jnp.float32) * scale


def setup_inputs(seed: int = 0) -> dict:
    key = jax.random.key(seed)
    ks = jax.random.split(key, 20)
    L, D, F = DEPTH, D_MODEL, FFN_HIDDEN
    return {
        "x": _normal(ks[0], (BATCH, SEQ, D), 1.0),
        "mem": _normal(ks[1], (BATCH, MEM_LEN, D), 1.0),
        "norm_mix_pre": 1.0 + _normal(ks[2], (L, D), 0.05),
        "norm_mix_post": 1.0 + _normal(ks[3], (L, D), 0.05),
        "norm_ffn_pre": 1.0 + _normal(ks[4], (L, D), 0.05),
        "norm_ffn_post": 1.0 + _normal(ks[5], (L, D), 0.05),
        "norm_mem": 1.0 + _normal(ks[6], (L, D), 0.05),
        "w_in": _normal(ks[7], (L, D, IN_WIDTH), D ** -0.5),
        "rel_bias": _normal(ks[8], (NUM_BUCKETS, N_SELF_HEADS), 0.3),
        "swa_sinks": _normal(ks[9], (L, SWA_Q_HEADS), 1.0),
        "w_mem_kv": _normal(ks[10], (L, D, 2 * MEM_W), D ** -0.5),
        "w_branch_moba": _normal(ks[11], (L, MOBA_W, D), MOBA_W ** -0.5),
        "w_branch_swa": _normal(ks[12], (L, SWA_QW, D), SWA_QW ** -0.5),
        "w_branch_mem": _normal(ks[13], (L, MEM_W, D), MEM_W ** -0.5),
        "w_out": _normal(ks[14], (L, D, D), D ** -0.5),
        "w_ffn_up": _normal(ks[15], (L, D, 2 * F), D ** -0.5),
        "ffn_conv_w": _normal(ks[16], (L, CONV_WIDTH, 2 * F), CONV_WIDTH ** -0.5),
        "ffn_conv_b": _normal(ks[17], (L, 2 * F), 0.02),
        "w_ffn_down": _normal(ks[18], (L, F, D), F ** -0.5),
    }


def reference(x, mem, norm_mix_pre, norm_mix_post, norm_ffn_pre, norm_ffn_post, norm_mem,
              w_in, rel_bias, swa_sinks, w_mem_kv, w_branch_moba, w_branch_swa, w_branch_mem,
              w_out, w_ffn_up, ffn_conv_w, ffn_conv_b, w_ffn_down):
    B_, S_, _ = x.shape
    cuts = []
    acc = 0
    for width in IN_SPLITS[:-1]:
        acc += width
        cuts.append(acc)
    bias_moba = rel_bias[:, :MOBA_HEADS].T
    bias_swa = rel_bias[:, MOBA_HEADS:].T
    for l in range(DEPTH):
        h = rmsnorm(x, norm_mix_pre[l])
        proj = h @ w_in[l]
        (q_mb, k_mb, v_mb, q_sw, k_sw, v_sw, q_me,
         g_mb, g_sw, g_me) = jnp.split(proj, cuts, axis=-1)
        o_mb = moba_attention(
            q_mb.reshape(B_, S_, MOBA_HEADS, MOBA_HEAD_DIM),
            k_mb.reshape(B_, S_, MOBA_HEADS, MOBA_HEAD_DIM),
            v_mb.reshape(B_, S_, MOBA_HEADS, MOBA_HEAD_DIM), bias_moba).reshape(B_, S_, MOBA_W)
        o_sw = swa_attention(
            q_sw.reshape(B_, S_, SWA_Q_HEADS, SWA_HEAD_DIM),
            k_sw.reshape(B_, S_, SWA_KV_HEADS, SWA_HEAD_DIM),
            v_sw.reshape(B_, S_, SWA_KV_HEADS, SWA_HEAD_DIM), swa_sinks[l], bias_swa)
        kv_me = rmsnorm(mem, norm_mem[l]) @ w_mem_kv[l]
        M_ = mem.shape[1]
        k_me = kv_me[..., :MEM_W].reshape(B_, M_, MEM_HEADS, MEM_HEAD_DIM)
        v_me = kv_me[..., MEM_W:].reshape(B_, M_, MEM_HEADS, MEM_HEAD_DIM)
        o_me = memory_attention(q_me.reshape(B_, S_, MEM_HEADS, MEM_HEAD_DIM), k_me, v_me)
        merged = (jax.nn.sigmoid(g_mb) * (o_mb @ w_branch_moba[l])
                  + jax.nn.sigmoid(g_sw) * (o_sw @ w_branch_swa[l])
                  + jax.nn.sigmoid(g_me) * (o_me @ w_branch_mem[l]))
        x = x + rmsnorm(merged @ w_out[l], norm_mix_post[l])
        h = rmsnorm(x, norm_ffn_pre[l])
        u = causal_depthwise_conv(h @ w_ffn_up[l], ffn_conv_w[l], ffn_conv_b[l])
        u_gate, u_val = jnp.split(u, 2, axis=-1)
        y = (jax.nn.gelu(u_gate, approximate=True) * u_val) @ w_ffn_down[l]
        x = x + rmsnorm(y, norm_ffn_post[l])
    return x
```

```python
import math
import os
import numpy as np
import ml_dtypes
import concourse.bass as bass
import concourse.mybir as mybir
from concourse.bass_utils import run_bass_kernel_spmd

F32 = mybir.dt.float32
BF16 = mybir.dt.bfloat16
AF = mybir.ActivationFunctionType
ALU = mybir.AluOpType
AX = mybir.AxisListType

D = 1024
FH = 2816
NEG = -30000.0


class Prog:
    ENGS = ['pe', 'act', 'dve', 'pool', 'sp']

    def __init__(self, nc):
        self.nc = nc
        self.stream = {e: [] for e in self.ENGS}
        self.semval = {}
        self.sems = {}
        self.waited = {}
        self.res = {}
        self.lastwait = {}
        self.nops = 0
        for e in self.ENGS:
            self._sem('c_' + e)

    def _sem(self, name):
        if name not in self.sems:
            self.sems[name] = self.nc.alloc_semaphore(name)
            self.semval[name] = 0
        return name

    def op(self, e, fn, reads=(), writes=(), dma=None):
        deps = {}
        self.nops += 1

        def add(s, v):
            if deps.get(s, 0) < v:
                deps[s] = v
        for r in reads:
            st = self.res.get(r)
            if st and st['w']:
                add(*st['w'])
        for w in writes:
            st = self.res.get(w)
            if st:
                if st['w']:
                    add(*st['w'])
                for s, v in st['r'].items():
                    add(s, v)
        if dma is None:
            sem, inc = 'c_' + e, 1
        else:
            sem, inc = self._sem('d_' + dma), 16
        waits = []
        for s, v in deps.items():
            if e == 'pe' and s == 'c_pe':
                continue
            if s.startswith('d_'):
                v = self.semval[s]
                self.lastwait[s] = max(self.lastwait.get(s, 0), v)
            if self.waited.get((e, s), 0) >= v:
                continue
            self.waited[(e, s)] = v
            waits.append((s, v))
        if dma is not None:
            lw = self.lastwait.get(sem, 0)
            if lw > self.waited.get((e, sem), 0):
                self.waited[(e, sem)] = lw
                waits.append((sem, lw))
        self.semval[sem] += inc
        tok = (sem, self.semval[sem])
        self.stream[e].append((waits, fn, sem, inc))
        for r in reads:
            st = self.res.setdefault(r, dict(w=None, r={}))
            st['r'][sem] = tok[1]
        for w in writes:
            self.res[w] = dict(w=tok, r={})
        return tok

    def wait_all(self, e, names):
        waits = [('d_' + s, self.semval['d_' + s]) for s in names if ('d_' + s) in self.semval]
        self.stream[e].append((waits, None, None, 0))

    def emit(self):
        nc = self.nc
        with nc.Block() as block:
            def mk(e):
                def body(eng):
                    for waits, fn, sem, inc in self.stream[e]:
                        for s, v in waits:
                            eng.wait_ge(self.sems[s], v)
                        if fn is not None:
                            fn(eng).then_inc(self.sems[sem], inc)
                return body
            block.tensor(mk('pe'))
            block.scalar(mk('act'))
            block.vector(mk('dve'))
            block.gpsimd(mk('pool'))
            block.sync(mk('sp'))


def build(NB, C0):
    NG = NB - C0
    NT = NB * 2
    nc = bass.Bass("TRN2", target_bir_lowering=False)

    def din(name, shape, dt=F32):
        return nc.dram_tensor(name, shape, dt, kind="ExternalInput").ap()
    xv = din("xv", [NB * 256, D])
    memx = din("mem", [256, D])
    w_in = din("w_in", [D, 5888])
    w_mkv = din("w_mem_kv", [D, 1024])
    w_br = [din("w_br%d" % i, [512, D]) for i in range(3)]
    w_out = din("w_out", [D, D])
    w_up = din("w_up", [D, 2 * FH])
    w_dn = din("w_dn", [FH, D])
    gpre_d = din("gpre", [128, 3, 8])
    gpost_d = din("gpost", [2, D])
    cw_d = din("cw", [128, 4, 44])
    biasT_d = din("biasT", [128, 16, 2, 128])
    maskT_d = din("maskT", [128, 16, 2, 128])
    relb_d = din("rel_bias", [32, 16])
    sinks_d = din("sinks", [1, 8])
    gmask_d = din("gmask", [1, NG * 32])
    fmask_d = din("firstmask", [128, 128])
    hflag_d = din("haloflag", [128, 1])
    outd = nc.dram_tensor("out", [(NB // 2) * 256, D], F32, kind="ExternalOutput").ap()
    win_b = nc.dram_tensor("win_b", [D, 5888], BF16).ap()
    wmkv_b = nc.dram_tensor("wmkv_b", [D, 1024], BF16).ap()
    wbr_b = [nc.dram_tensor("wbr_b%d" % i, [512, D], BF16).ap() for i in range(3)]
    wout_b = nc.dram_tensor("wout_b", [D, D], BF16).ap()
    wup_b = nc.dram_tensor("wup_b", [D, 2 * FH], BF16).ap()
    wdn_b = nc.dram_tensor("wdn_b", [FH, D], BF16).ap()
    vscr = nc.dram_tensor("vscr", [NT, 128, 520], BF16).ap()

    P = Prog(nc)
    STOP = int(os.environ.get('K_STOP', '0'))
    SKIP = os.environ.get('K_SKIP', '').split(',')

    class _Stop(Exception):
        pass

    def ckpt(k):
        if STOP == k:
            raise _Stop()
    sb = nc.alloc_sbuf_tensor
    KT = sb("KT", [128, 4, NB * 256], BF16)
    kmT = sb("kmT", [128, 4, 32], F32)
    kmTb = sb("kmTb", [128, 4, 32], BF16)
    KswT = sb("KswT", [128, 4, 128], BF16)
    Vsw = sb("Vsw", [128, 4, 2, 65], BF16)
    KmeT = sb("KmeT", [128, 4, 256], BF16)
    Vme = sb("Vme", [128, 2, 4, 129], BF16)
    bm = sb("bm", [128, 16, 2, 128], BF16)
    gpost = sb("gpost_s", [128, 2, D], F32)
    gpre = sb("gpre_s", [128, 3, 8], F32)
    cw = sb("cw_s", [128, 4, 44], F32)
    carry = sb("carry_s", [128, 44, 2], F32)
    ident = sb("ident", [128, 128], BF16)
    identf = sb("identf", [128, 128], F32)
    b31 = sb("b31", [128, 8], F32)
    expsink = sb("expsink", [128, 8], F32)
    gmask = sb("gmask_s", [128, NG, 32], F32)
    fmask = sb("fmask_s", [128, 128], F32)
    hflag = sb("hflag_s", [128, 1], F32)
    xt = sb("xt_s", [128, 2, D], F32)
    xs = sb("xs", [128, D], BF16)
    hT = sb("hT", [128, 8, 256], BF16)
    NSLAB = 3
    ws = [sb("ws%d" % i, [128, 4096], BF16) for i in range(NSLAB)]
    QTz = sb("QTz", [128, 8, 256], BF16)
    QswTz = sb("QswTz", [128, 8, 256], BF16)
    QmeT = sb("QmeT", [128, 4, 256], BF16)
    gs = [sb("gs%d" % i, [128, 256], F32) for i in range(2)]
    vown = sb("vown", [128, 2, 8, 65], BF16)
    vs = [sb("vs%d" % i, [128, 4, 8, 65], BF16) for i in range(2)]
    PT = [sb("PT%d" % i, [128, 512], BF16) for i in range(3)]
    tmp = [sb("tmp%d" % i, [128, 384], F32) for i in range(2)]
    acc = sb("acc", [128, 2, 8, 65], F32)
    gmt = sb("gmt", [128, 2, 8, 32], F32)
    top8 = sb("top8", [128, 8, 8], F32)
    thr = sb("thr", [128, 8], F32)
    msk = sb("msk", [128, 2, 8, 32], F32)
    rec = sb("rec", [128, 2, 8], F32)
    den = sb("den", [128, 2], F32)
    Otok = sb("Otok", [128, 2, 512], BF16)
    OT = sb("OT", [128, 4, 256], BF16)
    mergedf = sb("mergedf", [128, 8, 256], F32)
    tg = [sb("tg%d" % i, [128, 256], F32) for i in range(2)]
    mergedT = sb("mergedT", [128, 8, 256], BF16)
    ub = [sb("ub%d" % i, [128, 258], F32) for i in range(2)]
    tb = [sb("tb%d" % i, [128, 256], F32) for i in range(4)]
    gl = [sb("gl%d" % i, [128, 256], F32) for i in range(2)]
    actT = [sb("actT%d" % i, [128, 256], BF16) for i in range(4)]
    ss = sb("ss", [128, 4], F32)
    rr = sb("rr", [128, 2], F32)
    ps = nc.alloc_psum_tensor
    psA = [ps("psA%d" % i, [128, 512], F32) for i in range(2)]
    psS = [ps("psS%d" % i, [128, 512], F32) for i in range(2)]
    psO = [ps("psO%d" % i, [128, 512], F32) for i in range(2)]
    psT = ps("psT", [128, 1024], BF16)
    psG = ps("psG", [128, 512], F32)

    rot = {}

    def nxt(name, n):
        i = rot.get(name, 0)
        rot[name] = (i + 1) % n
        return i

    try:
        def castw(dst, src, rows, tag):
            for r0 in range(0, rows, 128):
                P.op('pool', lambda e, r0=r0: e.dma_start(out=dst[r0:r0 + 128, :], in_=src[r0:r0 + 128, :]),
                     writes=[tag], dma='cast')
        for r0 in range(0, D, 128):
            P.op('pool', lambda e, r0=r0: e.dma_start(out=win_b[r0:r0 + 128, 0:1536], in_=w_in[r0:r0 + 128, 0:1536]),
                 writes=['win_b'], dma='cast')
            P.op('pool', lambda e, r0=r0: e.dma_start(out=win_b[r0:r0 + 128, 2048:5888], in_=w_in[r0:r0 + 128, 2048:5888]),
                 writes=['win_b'], dma='cast')
            for two in range(2):
                P.op('pool', lambda e, r0=r0, two=two: e.dma_start(
                    out=win_b[r0:r0 + 128, 1536:2048].rearrange("r (j t c) -> r j t c", j=4, t=2)[:, :, two, :],
                    in_=w_in[r0:r0 + 128, 1536 + two * 256:1536 + (two + 1) * 256].rearrange("r (j c) -> r j c", j=4)),
                    writes=['win_b'], dma='cast')
        castw(wmkv_b, w_mkv, D, 'wmkv_b')
        for i in range(3):
            castw(wbr_b[i], w_br[i], 512, 'wbr_b%d' % i)
        castw(wout_b, w_out, D, 'wout_b')
        castw(wup_b, w_up, D, 'wup_b')
        castw(wdn_b, w_dn, FH, 'wdn_b')

        P.op('sp', lambda e: e.dma_start(out=gpre[:], in_=gpre_d[:, :, :]), writes=['gpre'], dma='c')
        P.op('sp', lambda e: e.dma_start(out=cw[:], in_=cw_d[:, :, :]), writes=['cw'], dma='c')
        for i in range(2):
            P.op('sp', lambda e, i=i: e.dma_start(out=gpost[:, i, :], in_=gpost_d[i:i + 1, :].broadcast_to([128, D])),
                 writes=['gpost'], dma='c')
        P.op('sp', lambda e: e.dma_start(out=b31[:], in_=relb_d[31:32, 0:8].broadcast_to([128, 8])), writes=['b31'], dma='c')
        P.op('sp', lambda e: e.dma_start(out=expsink[:], in_=sinks_d[0:1, :].broadcast_to([128, 8])), writes=['expsink'], dma='c')
        P.op('sp', lambda e: e.dma_start(out=gmask[:].rearrange("p g n -> p (g n)"), in_=gmask_d[0:1, :].broadcast_to([128, NG * 32])),
             writes=['gmask'], dma='c')
        P.op('sp', lambda e: e.dma_start(out=fmask[:], in_=fmask_d[:, :]), writes=['fmask'], dma='c')
        P.op('sp', lambda e: e.dma_start(out=hflag[:], in_=hflag_d[:, :]), writes=['hflag'], dma='c')
        P.op('act', lambda e: e.activation(out=expsink[:], in_=expsink[:], func=AF.Exp), reads=['expsink'], writes=['expsink'])
        xtf = xt[:].rearrange("p a b -> p (a b)")
        mff = mergedf[:].rearrange("p a b -> p (a b)")
        for hf in range(2):
            P.op('sp', lambda e, hf=hf: e.dma_start(out=xtf, in_=biasT_d[:, hf * 8:(hf + 1) * 8, :, :].rearrange("p a b c -> p (a b c)")),
                 writes=['xt0', 'xt1'], dma='c')
            P.op('sp', lambda e, hf=hf: e.dma_start(out=mff, in_=maskT_d[:, hf * 8:(hf + 1) * 8, :, :].rearrange("p a b c -> p (a b c)")),
                 writes=['mergedf'], dma='c')
            P.op('dve', lambda e, hf=hf: e.tensor_tensor(out=bm[:, hf * 8:(hf + 1) * 8, :, :].rearrange("p a b c -> p (a b c)"),
                                                         in0=xtf, in1=mff, op=ALU.add),
                 reads=['xt0', 'xt1', 'mergedf'], writes=['bm'])
        P.op('pool', lambda e: e.memset(identf[:], 0.0), writes=['identf'])
        P.op('pool', lambda e: e.affine_select(out=identf[:], in_=identf[:], pattern=[[-1, 128]], compare_op=ALU.not_equal,
                                               fill=1.0, base=0, channel_multiplier=1), reads=['identf'], writes=['identf'])
        P.op('dve', lambda e: e.tensor_copy(out=ident[:], in_=identf[:]), reads=['identf'], writes=['ident'])
        P.op('pool', lambda e: e.memset(kmT[:], 0.0), writes=['km'])
        P.op('pool', lambda e: e.memset(kmTb[:], 0.0), writes=['kmb'])
        P.op('pool', lambda e: e.memset(QTz[:], 0.0), writes=['QTz%d' % h for h in range(8)])
        P.op('pool', lambda e: e.memset(QswTz[:], 0.0), writes=['QswTz%d' % h for h in range(8)])
        P.op('pool', lambda e: e.memset(vown[:], 1.0), writes=['vown'])
        P.op('pool', lambda e: e.memset(Vsw[:], 1.0), writes=['Vsw%d' % i for i in range(4)])
        P.op('pool', lambda e: e.memset(Vme[:], 1.0), writes=['Vme'])
        P.op('pool', lambda e: e.memset(carry[:], 0.0), writes=['carry'])
        P.op('pool', lambda e: e.memset(KswT[:], 0.0), writes=['KswT%d' % i for i in range(4)])

        ckpt(1)
        def load_slab(parts):
            s = nxt('ws', NSLAB)
            for (src, nch, width, off, ncols, rd) in parts:
                dst = ws[s][:, 0:nch * width].rearrange("p (c n) -> p c n", c=nch)[:, :, off:off + ncols]
                P.op('sp', lambda e, dst=dst, src=src: e.dma_start(out=dst, in_=src.rearrange("(c p) n -> p c n", p=128)),
                     reads=[rd], writes=['ws%d' % s], dma='ws%d' % s)
            return s

        def slabv(s, nch, width):
            return ws[s][:, 0:nch * width].rearrange("p (c n) -> p c n", c=nch)

        def norm_T(srcs, gi, ncols):
            for i, (src, rs) in enumerate(srcs):
                P.op('act', lambda e, src=src, i=i: e.activation(out=xs[:], in_=src, func=AF.Square, accum_out=ss[:, i:i + 1]),
                     reads=[rs], writes=['xs', 'ss'])
                P.op('act', lambda e, i=i: e.activation(out=rr[:, i:i + 1], in_=ss[:, i:i + 1], func=AF.Sqrt, scale=1.0 / D, bias=1e-6),
                     reads=['ss'], writes=['rr'])
                P.op('dve', lambda e, i=i: e.reciprocal(out=rr[:, i:i + 1], in_=rr[:, i:i + 1]), reads=['rr'], writes=['rr'])
                P.op('dve', lambda e, src=src, i=i: e.tensor_scalar(out=xs[:], in0=src, scalar1=rr[:, i:i + 1], scalar2=None, op0=ALU.mult),
                     reads=[rs, 'rr'], writes=['xs'])
                for c in range(8):
                    P.op('pe', lambda e, c=c: e.transpose(out=psT[:, c * 128:(c + 1) * 128], in_=xs[:, c * 128:(c + 1) * 128], identity=ident[:]),
                         reads=['xs', 'ident'], writes=['psT'])
                for c in range(8):
                    eng = 'act'
                    if eng == 'act':
                        P.op('act', lambda e, c=c, i=i: e.activation(out=hT[:, c, i * 128:(i + 1) * 128], in_=psT[:, c * 128:(c + 1) * 128],
                                                                     func=AF.Identity, scale=gpre[:, gi, c:c + 1]),
                             reads=['psT', 'gpre'], writes=['hT'])
                    else:
                        P.op('dve', lambda e, c=c, i=i: e.tensor_scalar(out=hT[:, c, i * 128:(i + 1) * 128], in0=psT[:, c * 128:(c + 1) * 128],
                                                                        scalar1=gpre[:, gi, c:c + 1], scalar2=None, op0=ALU.mult),
                             reads=['psT', 'gpre'], writes=['hT'])

        def fm_chunk(s, nch, width, col, ntok, rhs_t=None, rhs_res='hT'):
            a = nxt('psA', 2)
            sv = slabv(s, nch, width)
            src = hT if rhs_t is None else rhs_t
            for d in range(nch):
                P.op('pe', lambda e, d=d: e.matmul(psA[a][:, 0:ntok], lhsT=sv[:, d, col:col + 128], rhs=src[:, d, 0:ntok],
                                                   start=(d == 0), stop=(d == nch - 1)),
                     reads=['ws%d' % s, rhs_res], writes=['psA%d' % a])
            return a

        def tm_tile(s, nch, width, col, ncols, sub):
            a = nxt('psA', 2)
            sv = slabv(s, nch, width)
            for d in range(nch):
                P.op('pe', lambda e, d=d: e.matmul(psA[a][:, 0:ncols], lhsT=hT[:, d, sub * 128:(sub + 1) * 128], rhs=sv[:, d, col:col + ncols],
                                                   start=(d == 0), stop=(d == nch - 1)),
                     reads=['ws%d' % s, 'hT'], writes=['psA%d' % a])
            return a

        def load_x(n):
            for sub in range(2):
                P.op('sp', lambda e, sub=sub: e.dma_start(out=xt[:, sub, :], in_=xv[n * 256 + sub * 128:n * 256 + (sub + 1) * 128, :]),
                     writes=['xt%d' % sub], dma='x')

        def proj_kv(n):
            s = load_slab([(win_b[:, 512:1024], 8, 512, 0, 512, 'win_b')])
            for p in range(4):
                a = fm_chunk(s, 8, 512, p * 128, 256)
                P.op('act', lambda e, a=a, p=p: e.activation(out=KT[:, p, n * 256:(n + 1) * 256], in_=psA[a][:, 0:256], func=AF.Identity,
                                                             accum_out=kmT[:, p, n:n + 1]),
                     reads=['psA%d' % a], writes=['KT%d' % n, 'km'])
            if 'kmb' not in SKIP:
              P.op('dve', lambda e: e.tensor_copy(out=kmTb[:, :, n:n + 1], in_=kmT[:, :, n:n + 1]), reads=['km'], writes=['kmb'])
            s = load_slab([(win_b[:, 1024:1536], 8, 512, 0, 512, 'win_b')])
            for sub in range(2):
                a = tm_tile(s, 8, 512, 0, 512, sub)
                P.op('dve', lambda e, a=a, sub=sub: e.tensor_copy(out=vown[:, sub, :, 0:64], in_=psA[a][:, 0:512].rearrange("p (h c) -> p h c", h=8)),
                     reads=['psA%d' % a], writes=['vown'])
            for sub in range(2 if 'vst' not in SKIP else 0):
                P.op('pool', lambda e, sub=sub: e.dma_start(out=vscr[2 * n + sub, :, :], in_=vown[:, sub, :, :].rearrange("p h c -> p (h c)")),
                     reads=['vown'], writes=['vscr%d' % n], dma='vst')
            s = load_slab([(win_b[:, 2048:2304], 8, 256, 0, 256, 'win_b')])
            a = fm_chunk(s, 8, 256, 0, 256)
            for sub in range(2):
                sl = (2 * n + sub) % 4
                P.op('act', lambda e, a=a, sub=sub, sl=sl: e.copy(out=KswT[:, sl, :], in_=psA[a][:, sub * 128:(sub + 1) * 128]),
                     reads=['psA%d' % a], writes=['KswT%d' % sl])
            for sub in range(2):
                sl = (2 * n + sub) % 4
                a = tm_tile(s, 8, 256, 128, 128, sub)
                P.op('dve', lambda e, a=a, sl=sl: e.tensor_copy(out=Vsw[:, sl, :, 0:64], in_=psA[a][:, 0:128].rearrange("p (h c) -> p h c", h=2)),
                     reads=['psA%d' % a], writes=['Vsw%d' % sl])

        def otok_to_OT():
            for sub in range(2):
                for pc in range(4):
                    P.op('pe', lambda e, sub=sub, pc=pc: e.transpose(out=psT[:, pc * 128:(pc + 1) * 128], in_=Otok[:, sub, pc * 128:(pc + 1) * 128], identity=ident[:]),
                         reads=['Otok', 'ident'], writes=['psT'])
                P.op('act', lambda e, sub=sub: e.copy(out=OT[:, :, sub * 128:(sub + 1) * 128], in_=psT[:, 0:512].rearrange("p (c n) -> p c n", c=4)),
                     reads=['psT'], writes=['OT'])

        def branch_merge(b, pos):
            sb_ = load_slab([(wbr_b[b][:, :], 4, 1024, 0, 1024, 'wbr_b%d' % b)])
            bv = slabv(sb_, 4, 1024)
            for nh in range(2):
                sg = load_slab([(win_b[:, 2816 + b * 1024 + nh * 512:2816 + b * 1024 + (nh + 1) * 512], 8, 512, 0, 512, 'win_b')])
                for n4 in range(4):
                    ncx = nh * 4 + n4
                    a = fm_chunk(sg, 8, 512, n4 * 128, 256)
                    g = nxt('gs', 2)
                    P.op('act', lambda e, a=a, g=g: e.activation(out=gs[g][:], in_=psA[a][:, 0:256], func=AF.Sigmoid),
                         reads=['psA%d' % a], writes=['gs%d' % g])
                    hfx = nxt('psS', 2)
                    for hc in range(4):
                        P.op('pe', lambda e, hc=hc, hfx=hfx, ncx=ncx: e.matmul(psS[hfx][:, 0:256], lhsT=bv[:, hc, ncx * 128:(ncx + 1) * 128],
                                                                              rhs=OT[:, hc, :], start=(hc == 0), stop=(hc == 3)),
                             reads=['ws%d' % sb_, 'OT'], writes=['psS%d' % hfx])
                    if pos == 'first':
                        P.op('dve', lambda e, g=g, hfx=hfx, ncx=ncx: e.tensor_tensor(out=mergedf[:, ncx, :], in0=psS[hfx][:, 0:256], in1=gs[g][:], op=ALU.mult),
                             reads=['psS%d' % hfx, 'gs%d' % g], writes=['mergedf'])
                    else:
                        t = nxt('tg', 2)
                        P.op('dve', lambda e, g=g, hfx=hfx, t=t: e.tensor_tensor(out=tg[t][:], in0=psS[hfx][:, 0:256], in1=gs[g][:], op=ALU.mult),
                             reads=['psS%d' % hfx, 'gs%d' % g], writes=['tg%d' % t])
                        if pos == 'mid':
                            P.op('pool', lambda e, t=t, ncx=ncx: e.tensor_tensor(out=mergedf[:, ncx, :], in0=mergedf[:, ncx, :], in1=tg[t][:], op=ALU.add),
                                 reads=['tg%d' % t, 'mergedf'], writes=['mergedf'])
                        else:
                            P.op('pool', lambda e, t=t, ncx=ncx: e.tensor_tensor(out=mergedT[:, ncx, :], in0=mergedf[:, ncx, :], in1=tg[t][:], op=ALU.add),
                                 reads=['tg%d' % t, 'mergedf'], writes=['mergedT'])

        def post_norm_residual(pi, which, sub):
            pr = pi
            for nh in range(2):
                P.op('act', lambda e, nh=nh: e.activation(out=xs[:, nh * 512:(nh + 1) * 512], in_=pr[nh][0][:, 0:512], func=AF.Square,
                                                          accum_out=ss[:, 2 + nh:3 + nh]),
                     reads=[pr[nh][1]], writes=['xs', 'ss'])
            P.op('dve', lambda e: e.tensor_tensor(out=ss[:, 0:1], in0=ss[:, 2:3], in1=ss[:, 3:4], op=ALU.add), reads=['ss'], writes=['ss'])
            P.op('act', lambda e: e.activation(out=rr[:, 0:1], in_=ss[:, 0:1], func=AF.Sqrt, scale=1.0 / D, bias=1e-6), reads=['ss'], writes=['rr'])
            P.op('dve', lambda e: e.reciprocal(out=rr[:, 0:1], in_=rr[:, 0:1]), reads=['rr'], writes=['rr'])
            mv = mergedf[:].rearrange("p a b -> p (a b)")
            for nh in range(2):
                P.op('dve', lambda e, nh=nh: e.scalar_tensor_tensor(out=mv[:, sub * 1024 + nh * 512:sub * 1024 + (nh + 1) * 512], in0=pr[nh][0][:, 0:512],
                                                                   scalar=rr[:, 0:1], in1=gpost[:, which, nh * 512:(nh + 1) * 512],
                                                                   op0=ALU.mult, op1=ALU.mult),
                     reads=[pr[nh][1], 'rr', 'gpost'], writes=['mergedf'])
            P.op('pool', lambda e: e.tensor_tensor(out=xt[:, sub, :], in0=xt[:, sub, :], in1=mv[:, sub * 1024:(sub + 1) * 1024], op=ALU.add),
                 reads=['mergedf', 'xt%d' % sub], writes=['xt%d' % sub])

        for sub in range(2):
            P.op('sp', lambda e, sub=sub: e.dma_start(out=xt[:, sub, :], in_=memx[sub * 128:(sub + 1) * 128, :]),
                 writes=['xt%d' % sub], dma='x')
        norm_T([(xt[:, 0, :], 'xt0'), (xt[:, 1, :], 'xt1')], 2, 256)
        s = load_slab([(wmkv_b[:, 0:512], 8, 512, 0, 512, 'wmkv_b')])
        for h in range(4):
            a = fm_chunk(s, 8, 512, h * 128, 256)
            P.op('act', lambda e, a=a, h=h: e.copy(out=KmeT[:, h, :], in_=psA[a][:, 0:256]), reads=['psA%d' % a], writes=['KmeT'])
        s = load_slab([(wmkv_b[:, 512:1024], 8, 512, 0, 512, 'wmkv_b')])
        for sub in range(2):
            a = tm_tile(s, 8, 512, 0, 512, sub)
            P.op('dve', lambda e, a=a, sub=sub: e.tensor_copy(out=Vme[:, sub, :, 0:128], in_=psA[a][:, 0:512].rearrange("p (h c) -> p h c", h=4)),
                 reads=['psA%d' % a], writes=['Vme'])

        ckpt(2)
        for n in range(C0):
            load_x(n)
            norm_T([(xt[:, 0, :], 'xt0'), (xt[:, 1, :], 'xt1')], 0, 256)
            proj_kv(n)

        ckpt(3)
        SC = 0.125
        SCM = 1.0 / math.sqrt(128.0)
        def main_group(gi):
            c = C0 + gi
            load_x(c)
            norm_T([(xt[:, 0, :], 'xt0'), (xt[:, 1, :], 'xt1')], 0, 256)
            s = load_slab([(win_b[:, 0:512], 8, 512, 0, 512, 'win_b')])
            for p in range(4):
                a = fm_chunk(s, 8, 512, p * 128, 256)
                P.op('act', lambda e, a=a, p=p: e.copy(out=QTz[0:64, 2 * p, :], in_=psA[a][0:64, 0:256]),
                     reads=['psA%d' % a], writes=['QTz%d' % (2 * p)])
                P.op('dve', lambda e, a=a, p=p: e.tensor_copy(out=QTz[64:128, 2 * p + 1, :], in_=psA[a][64:128, 0:256]),
                     reads=['psA%d' % a], writes=['QTz%d' % (2 * p + 1)])
            proj_kv(c)
            s = load_slab([(win_b[:, 1536:2048], 8, 512, 0, 512, 'win_b')])
            for j in range(4):
                a = fm_chunk(s, 8, 512, j * 128, 256)
                P.op('act', lambda e, a=a, j=j: e.copy(out=QswTz[0:64, j, :], in_=psA[a][0:64, 0:256]),
                     reads=['psA%d' % a], writes=['QswTz%d' % j])
                P.op('dve', lambda e, a=a, j=j: e.tensor_copy(out=QswTz[64:128, j + 4, :], in_=psA[a][64:128, 0:256]),
                     reads=['psA%d' % a], writes=['QswTz%d' % (j + 4)])
            s = load_slab([(win_b[:, 2304:2816], 8, 512, 0, 512, 'win_b')])
            for h in range(4):
                a = fm_chunk(s, 8, 512, h * 128, 256)
                P.op('act', lambda e, a=a, h=h: e.copy(out=QmeT[:, h, :], in_=psA[a][:, 0:256]), reads=['psA%d' % a], writes=['QmeT'])

            ckpt(4)
            for h in range(4):
                si = nxt('psS', 2)
                for mt in range(2):
                    P.op('pe', lambda e, h=h, mt=mt, si=si: e.matmul(psS[si][:, mt * 256:(mt + 1) * 256], lhsT=KmeT[:, h, mt * 128:(mt + 1) * 128],
                                                                    rhs=QmeT[:, h, :], start=True, stop=True),
                         reads=['KmeT', 'QmeT'], writes=['psS%d' % si])
                pi = nxt('PT', 3)
                P.op('act', lambda e, si=si, pi=pi: e.activation(out=PT[pi][:], in_=psS[si][:], func=AF.Exp, scale=SCM),
                     reads=['psS%d' % si], writes=['PT%d' % pi])
                oi = nxt('psO', 2)
                for sub in range(2):
                    for mt in range(2):
                        P.op('pe', lambda e, h=h, mt=mt, sub=sub, pi=pi, oi=oi: e.matmul(
                            psO[oi][:, sub * 129:(sub + 1) * 129], lhsT=PT[pi][:, mt * 256 + sub * 128:mt * 256 + (sub + 1) * 128],
                            rhs=Vme[:, mt, h, :], start=(mt == 0), stop=(mt == 1)),
                            reads=['PT%d' % pi, 'Vme'], writes=['psO%d' % oi])
                ov = psO[oi][:, 0:258].rearrange("p (s c) -> p s c", s=2)
                P.op('dve', lambda e, ov=ov: e.reciprocal(out=den[:, 0:2], in_=ov[:, :, 128]), reads=['psO%d' % oi], writes=['den'])
                for sub in range(2):
                    P.op('dve', lambda e, ov=ov, sub=sub, h=h: e.tensor_scalar(out=Otok[:, sub, h * 128:(h + 1) * 128], in0=ov[:, sub, 0:128],
                                                                             scalar1=den[:, sub:sub + 1], scalar2=None, op0=ALU.mult),
                         reads=['psO%d' % oi, 'den'], writes=['Otok'])
            otok_to_OT()
            branch_merge(2, 'first')

            ckpt(5)
            for sub in range(2):
                t = 2 * c + sub
                slp, slo = (t - 1) % 4, t % 4
                for h in range(8):
                    g = h // 4
                    si = nxt('psS', 2)
                    for o, sl in enumerate((slp, slo)):
                        P.op('pe', lambda e, h=h, o=o, sl=sl, si=si, sub=sub: e.matmul(psS[si][:, o * 128:(o + 1) * 128], lhsT=KswT[:, sl, :],
                                                                                     rhs=QswTz[:, h, sub * 128:(sub + 1) * 128], start=True, stop=True),
                             reads=['KswT%d' % sl, 'QswTz%d' % h], writes=['psS%d' % si])
                    ti = nxt('tmp', 2)
                    P.op('dve', lambda e, si=si, ti=ti, h=h: e.scalar_tensor_tensor(out=tmp[ti][:, 0:256], in0=psS[si][:, 0:256], scalar=SC,
                                                                                   in1=bm[:, 8 + h, :, :].rearrange("p a b -> p (a b)"),
                                                                                   op0=ALU.mult, op1=ALU.add),
                         reads=['psS%d' % si, 'bm'], writes=['tmp%d' % ti])
                    if gi == 1 and sub == 0:
                        P.op('dve', lambda e, ti=ti: e.tensor_tensor(out=tmp[ti][:, 0:128], in0=tmp[ti][:, 0:128], in1=fmask[:], op=ALU.add),
                             reads=['tmp%d' % ti, 'fmask'], writes=['tmp%d' % ti])
                    pi = nxt('PT', 3)
                    P.op('act', lambda e, ti=ti, pi=pi: e.activation(out=PT[pi][:, 0:256], in_=tmp[ti][:, 0:256], func=AF.Exp),
                         reads=['tmp%d' % ti], writes=['PT%d' % pi])
                    oi = nxt('psO', 2)
                    for o, sl in enumerate((slp, slo)):
                        P.op('pe', lambda e, o=o, sl=sl, pi=pi, oi=oi, g=g: e.matmul(psO[oi][:, 0:65], lhsT=PT[pi][:, o * 128:(o + 1) * 128],
                                                                                   rhs=Vsw[:, sl, g, :], start=(o == 0), stop=(o == 1)),
                             reads=['PT%d' % pi, 'Vsw%d' % sl], writes=['psO%d' % oi])
                    P.op('dve', lambda e, oi=oi, h=h: e.tensor_scalar(out=den[:, 0:1], in0=psO[oi][:, 64:65], scalar1=expsink[:, h:h + 1], scalar2=None, op0=ALU.add),
                         reads=['psO%d' % oi, 'expsink'], writes=['den'])
                    P.op('dve', lambda e: e.reciprocal(out=den[:, 0:1], in_=den[:, 0:1]), reads=['den'], writes=['den'])
                    P.op('dve', lambda e, oi=oi, h=h, sub=sub: e.tensor_scalar(out=Otok[:, sub, h * 64:(h + 1) * 64], in0=psO[oi][:, 0:64],
                                                                              scalar1=den[:, 0:1], scalar2=None, op0=ALU.mult),
                         reads=['psO%d' % oi, 'den'], writes=['Otok'])
            otok_to_OT()
            branch_merge(1, 'mid')

            ckpt(6)
            for sub in range(2 if 'topk' not in SKIP else 0):
                for h in range(8):
                    P.op('pe', lambda e, h=h, sub=sub: e.matmul(psG[:, h * 32:(h + 1) * 32], lhsT=QTz[:, h, sub * 128:(sub + 1) * 128],
                                                               rhs=kmTb[:, h // 2, :], start=True, stop=True),
                         reads=['QTz%d' % h, 'kmb'], writes=['psG'])
                for h in range(8):
                    P.op('dve', lambda e, h=h, sub=sub: e.tensor_tensor(out=gmt[:, sub, h, :], in0=psG[:, h * 32:(h + 1) * 32], in1=gmask[:, gi, :], op=ALU.add),
                         reads=['psG', 'gmask'], writes=['gmt'])
                for h in range(8):
                    P.op('dve', lambda e, h=h, sub=sub: e.max(out=top8[:, h, :], in_=gmt[:, sub, h, :]), reads=['gmt'], writes=['top8'])
                P.op('dve', lambda e: e.tensor_scalar(out=thr[:], in0=top8[:, :, 2], scalar1=-1e29, scalar2=None, op0=ALU.max),
                     reads=['top8'], writes=['thr'])
                for h in range(8):
                    P.op('dve', lambda e, h=h, sub=sub: e.tensor_scalar(out=msk[:, sub, h, :], in0=gmt[:, sub, h, :], scalar1=thr[:, h:h + 1], scalar2=None, op0=ALU.is_ge),
                         reads=['gmt', 'thr'], writes=['msk'])
            for h in range(8):
                p = h // 2
                si = nxt('psS', 2)
                P.op('pe', lambda e, h=h, p=p, si=si: e.matmul(psS[si][:, 0:256], lhsT=KT[:, p, (2 * c) * 128:(2 * c + 1) * 128], rhs=QTz[:, h, :], start=True, stop=True),
                     reads=['KT%d' % c, 'QTz%d' % h], writes=['psS%d' % si])
                P.op('pe', lambda e, h=h, p=p, si=si: e.matmul(psS[si][:, 256:384], lhsT=KT[:, p, (2 * c + 1) * 128:(2 * c + 2) * 128], rhs=QTz[:, h, 128:256], start=True, stop=True),
                     reads=['KT%d' % c, 'QTz%d' % h], writes=['psS%d' % si])
                ti = nxt('tmp', 2)
                P.op('dve', lambda e, si=si, ti=ti, h=h: e.scalar_tensor_tensor(out=tmp[ti][:, 0:256], in0=psS[si][:, 0:256], scalar=SC,
                                                                               in1=bm[:, h, :, :].rearrange("p a b -> p (a b)"), op0=ALU.mult, op1=ALU.add),
                     reads=['psS%d' % si, 'bm'], writes=['tmp%d' % ti])
                P.op('dve', lambda e, si=si, ti=ti, h=h: e.scalar_tensor_tensor(out=tmp[ti][:, 256:384], in0=psS[si][:, 256:384], scalar=SC,
                                                                               in1=bm[:, h, 0, :], op0=ALU.mult, op1=ALU.add),
                     reads=['psS%d' % si, 'bm'], writes=['tmp%d' % ti])
                pi = nxt('PT', 3)
                P.op('act', lambda e, ti=ti, pi=pi: e.activation(out=PT[pi][:, 0:384], in_=tmp[ti][:, 0:384], func=AF.Exp),
                     reads=['tmp%d' % ti], writes=['PT%d' % pi])
                oi = nxt('psO', 2)
                P.op('pe', lambda e, pi=pi, oi=oi, h=h: e.matmul(psO[oi][:, 0:65], lhsT=PT[pi][:, 0:128], rhs=vown[:, 0, h, :], start=True, stop=True),
                     reads=['PT%d' % pi, 'vown'], writes=['psO%d' % oi])
                P.op('pe', lambda e, pi=pi, oi=oi, h=h: e.matmul(psO[oi][:, 65:130], lhsT=PT[pi][:, 128:256], rhs=vown[:, 0, h, :], start=True, stop=False),
                     reads=['PT%d' % pi, 'vown'], writes=['psO%d' % oi])
                P.op('pe', lambda e, pi=pi, oi=oi, h=h: e.matmul(psO[oi][:, 65:130], lhsT=PT[pi][:, 256:384], rhs=vown[:, 1, h, :], start=False, stop=True),
                     reads=['PT%d' % pi, 'vown'], writes=['psO%d' % oi])
                P.op('dve', lambda e, oi=oi, h=h: e.tensor_copy(out=acc[:, :, h, :], in_=psO[oi][:, 0:130].rearrange("p (s c) -> p s c", s=2)),
                     reads=['psO%d' % oi], writes=['acc%d' % h])
            for s0 in range(0, c if 'past' not in SKIP else 0, 2):
                s1 = min(s0 + 2, c)
                vi = nxt('vs', 2)
                if 'vsl' not in SKIP:
                  P.op('pool', lambda e, vi=vi, s0=s0, s1=s1: e.dma_start(out=vs[vi][:, 0:2 * (s1 - s0), :, :].rearrange("p t h c -> p t (h c)"),
                                                                    in_=vscr[2 * s0:2 * s1, :, :].rearrange("t p c -> p t c")),
                     reads=['vscr%d' % n for n in range(s0, s1)], writes=['vs%d' % vi], dma='vs%d' % vi)
                for h in range(8):
                    p = h // 2
                    for n in range(s0, s1):
                        si = nxt('psS', 2)
                        for kt in range(2):
                            P.op('pe', lambda e, h=h, p=p, n=n, kt=kt, si=si: e.matmul(psS[si][:, kt * 256:(kt + 1) * 256], lhsT=KT[:, p, (2 * n + kt) * 128:(2 * n + kt + 1) * 128],
                                                                                     rhs=QTz[:, h, :], start=True, stop=True),
                                 reads=['KT%d' % n, 'QTz%d' % h], writes=['psS%d' % si])
                        pi = nxt('PT', 3)
                        if n == c - 1 and 'spec' not in SKIP:
                            ti = nxt('tmp', 2)
                            P.op('dve', lambda e, si=si, ti=ti, h=h: e.scalar_tensor_tensor(out=tmp[ti][:, 0:128], in0=psS[si][:, 256:384], scalar=SC,
                                                                                           in1=bm[:, h, 1, :], op0=ALU.mult, op1=ALU.add),
                                 reads=['psS%d' % si, 'bm'], writes=['tmp%d' % ti, 'psS%d' % si])
                            P.op('act', lambda e, si=si, pi=pi, h=h: e.activation(out=PT[pi][:, 0:256], in_=psS[si][:, 0:256], func=AF.Exp, scale=SC, bias=b31[:, h:h + 1]),
                                 reads=['psS%d' % si, 'b31'], writes=['PT%d' % pi])
                            P.op('act', lambda e, si=si, pi=pi, h=h: e.activation(out=PT[pi][:, 384:512], in_=psS[si][:, 384:512], func=AF.Exp, scale=SC, bias=b31[:, h:h + 1]),
                                 reads=['psS%d' % si, 'b31'], writes=['PT%d' % pi])
                            P.op('act', lambda e, ti=ti, pi=pi: e.activation(out=PT[pi][:, 256:384], in_=tmp[ti][:, 0:128], func=AF.Exp),
                                 reads=['tmp%d' % ti], writes=['PT%d' % pi])
                        else:
                            if 'nob31' in SKIP:
                                P.op('act', lambda e, si=si, pi=pi, h=h: e.activation(out=PT[pi][:], in_=psS[si][:], func=AF.Exp, scale=SC),
                                     reads=['psS%d' % si], writes=['PT%d' % pi])
                            else:
                              P.op('act', lambda e, si=si, pi=pi, h=h: e.activation(out=PT[pi][:], in_=psS[si][:], func=AF.Exp, scale=SC, bias=b31[:, h:h + 1]),
                                 reads=['psS%d' % si, 'b31'], writes=['PT%d' % pi])
                        oi = nxt('psO', 2)
                        for sub in range(2):
                            for kt in range(2):
                                P.op('pe', lambda e, pi=pi, oi=oi, sub=sub, kt=kt, vi=vi, n=n, h=h, s0=s0: e.matmul(
                                    psO[oi][:, sub * 65:(sub + 1) * 65], lhsT=PT[pi][:, kt * 256 + sub * 128:kt * 256 + (sub + 1) * 128],
                                    rhs=(vown[:, kt, h, :] if 'novs' in SKIP else vs[vi][:, 2 * (n - s0) + kt, h, :]), start=(kt == 0), stop=(kt == 1)),
                                    reads=['PT%d' % pi, 'vs%d' % vi], writes=['psO%d' % oi])
                        for sub in range(2 if 'stt' not in SKIP else 0):
                            P.op('dve', lambda e, oi=oi, sub=sub, h=h, n=n: e.scalar_tensor_tensor(out=acc[:, sub, h, :], in0=psO[oi][:, sub * 65:(sub + 1) * 65],
                                                                                                 scalar=msk[:, sub, h, n:n + 1], in1=acc[:, sub, h, :],
                                                                                                 op0=ALU.mult, op1=ALU.add),
                                 reads=['psO%d' % oi, 'msk', 'acc%d' % h], writes=['acc%d' % h])
            if 'mnorm' in SKIP:
                ckpt(7)
            P.op('dve', lambda e: e.reciprocal(out=rec[:], in_=acc[:, :, :, 64]), reads=['acc%d' % h for h in range(8)], writes=['rec'])
            for sub in range(2):
                P.op('dve', lambda e, sub=sub: e.tensor_tensor(out=Otok[:, sub, :].rearrange("p (h c) -> p h c", h=8), in0=acc[:, sub, :, 0:64],
                                                              in1=rec[:, sub, :].unsqueeze(2).broadcast_to([128, 8, 64]), op=ALU.mult),
                     reads=['acc%d' % h for h in range(8)] + ['rec'], writes=['Otok'])
            otok_to_OT()
            branch_merge(0, 'last')

            ckpt(7)
            so = [load_slab([(wout_b[:, nh * 512:(nh + 1) * 512], 8, 512, 0, 512, 'wout_b')]) for nh in range(2)]
            for sub in range(2):
                for nh in range(2):
                    sv = slabv(so[nh], 8, 512)
                    for d in range(8):
                        P.op('pe', lambda e, d=d, nh=nh, sub=sub, sv=sv: e.matmul(psA[nh][:, 0:512], lhsT=mergedT[:, d, sub * 128:(sub + 1) * 128], rhs=sv[:, d, :],
                                                                                start=(d == 0), stop=(d == 7)),
                             reads=['ws%d' % so[nh], 'mergedT'], writes=['psA%d' % nh])
                post_norm_residual([(psA[0], 'psA0'), (psA[1], 'psA1')], 0, sub)

            ckpt(8)
            norm_T([(xt[:, 0, :], 'xt0'), (xt[:, 1, :], 'xt1')], 1, 256)
            dacc = [[(psS[0], 'psS0'), (psS[1], 'psS1')], [(psO[0], 'psO0'), (psO[1], 'psO1')]]
            for j in range(11):
                s = load_slab([(wup_b[:, 256 * j:256 * (j + 1)], 8, 512, 0, 256, 'wup_b'),
                               (wup_b[:, FH + 256 * j:FH + 256 * (j + 1)], 8, 512, 256, 256, 'wup_b')])
                if gi > 0:
                    sd = load_slab([(wdn_b[256 * j:256 * (j + 1), :], 2, 1024, 0, 1024, 'wdn_b')])
                    dv = slabv(sd, 2, 1024)
                for cc in range(2):
                    tbi = []
                    for part in range(2):
                        ch = part * 22 + 2 * j + cc
                        a = fm_chunk(s, 8, 512, part * 256 + cc * 128, 256)
                        if gi == 0:
                            P.op('dve', lambda e, a=a, ch=ch: e.tensor_scalar(out=carry[:, ch, :], in0=psA[a][:, 254:256], scalar1=hflag[:, 0:1], scalar2=None, op0=ALU.mult),
                                 reads=['psA%d' % a, 'hflag'], writes=['carry'])
                            continue
                        u = nxt('ub', 2)
                        t = nxt('tb', 4)
                        tbi.append(t)
                        P.op('act', lambda e, a=a, u=u: e.copy(out=ub[u][:, 2:258], in_=psA[a][:, 0:256]), reads=['psA%d' % a], writes=['ub%d' % u])
                        P.op('pool', lambda e, u=u, ch=ch: e.tensor_copy(out=ub[u][:, 0:2], in_=carry[:, ch, :]), reads=['carry'], writes=['ub%d' % u])
                        P.op('act', lambda e, a=a, t=t, ch=ch: e.activation(out=tb[t][:], in_=psA[a][:, 0:256], func=AF.Identity,
                                                                            scale=cw[:, 2, ch:ch + 1], bias=cw[:, 3, ch:ch + 1]),
                             reads=['psA%d' % a, 'cw'], writes=['tb%d' % t])
                        P.op('dve', lambda e, u=u, t=t, ch=ch: e.scalar_tensor_tensor(out=tb[t][:], in0=ub[u][:, 1:257], scalar=cw[:, 1, ch:ch + 1], in1=tb[t][:],
                                                                                     op0=ALU.mult, op1=ALU.add),
                             reads=['ub%d' % u, 'tb%d' % t, 'cw'], writes=['tb%d' % t])
                        P.op('dve', lambda e, u=u, t=t, ch=ch: e.scalar_tensor_tensor(out=tb[t][:], in0=ub[u][:, 0:256], scalar=cw[:, 0, ch:ch + 1], in1=tb[t][:],
                                                                                     op0=ALU.mult, op1=ALU.add),
                             reads=['ub%d' % u, 'tb%d' % t, 'cw'], writes=['tb%d' % t])
                        P.op('pool', lambda e, u=u, ch=ch: e.tensor_copy(out=carry[:, ch, :], in_=ub[u][:, 256:258]), reads=['ub%d' % u], writes=['carry'])
                    if gi == 0:
                        continue
                    gli = nxt('gl', 2)
                    ai = nxt('actT', 4)
                    P.op('act', lambda e, gli=gli, t=tbi[0]: e.activation(out=gl[gli][:], in_=tb[t][:], func=AF.Gelu_apprx_tanh),
                         reads=['tb%d' % tbi[0]], writes=['gl%d' % gli])
                    P.op('dve', lambda e, gli=gli, ai=ai, t=tbi[1]: e.tensor_tensor(out=actT[ai][:], in0=gl[gli][:], in1=tb[t][:], op=ALU.mult),
                         reads=['gl%d' % gli, 'tb%d' % tbi[1]], writes=['actT%d' % ai])
                    fch = 2 * j + cc
                    for sub in range(2):
                        for nh in range(2):
                            P.op('pe', lambda e, ai=ai, sub=sub, nh=nh, cc=cc, dv=dv, fch=fch: e.matmul(
                                dacc[sub][nh][0][:, 0:512], lhsT=actT[ai][:, sub * 128:(sub + 1) * 128], rhs=dv[:, cc, nh * 512:(nh + 1) * 512],
                                start=(fch == 0), stop=(fch == 21)),
                                reads=['actT%d' % ai, 'ws%d' % sd], writes=[dacc[sub][nh][1]])
            if gi == 0:
                return
            for sub in range(2):
                post_norm_residual(dacc[sub], 1, sub)
                r0 = (c - C0 - 1) * 256 + sub * 128
                P.op('pool', lambda e, sub=sub, r0=r0: e.dma_start(out=outd[r0:r0 + 128, :], in_=xt[:, sub, :]),
                     reads=['xt%d' % sub], dma='out')
        for gi in range(NG):
            main_group(gi)
    except _Stop:
        pass
    P.wait_all('pool', ['out', 'cast', 'vst'])
    P.wait_all('sp', ['c', 'x', 'ws0', 'ws1', 'ws2', 'vs0', 'vs1'])
    P.emit()
    return nc


def _t5_bucket_np(dist):
    n = np.maximum(dist, 0)
    nf = np.maximum(n, 1).astype(np.float32)
    large = 16 + (np.log(nf / np.float32(16)) / np.float32(math.log(128 / 16)) * np.float32(16)).astype(np.int32)
    large = np.minimum(large, 31)
    return np.where(n < 16, n, large)


def _bias_tiles(rel_bias):
    k = np.arange(128)[:, None]
    q = np.arange(128)[None, :]
    biasT = np.zeros((128, 16, 2, 128), np.float32)
    maskT = np.zeros((128, 16, 2, 128), np.float32)
    for h in range(8):
        for o in range(2):
            dist = o * 128 + q - k
            ok = dist >= 0
            biasT[:, h, o, :] = np.where(ok, rel_bias[_t5_bucket_np(dist), h], np.float32(0))
            maskT[:, h, o, :] = np.where(ok, np.float32(0), np.float32(NEG))
        dist = 128 + q - k
        ok = dist < 128
        biasT[:, 8 + h, 0, :] = np.where(ok, rel_bias[_t5_bucket_np(dist), 8 + h], np.float32(0))
        maskT[:, 8 + h, 0, :] = np.where(ok, np.float32(0), np.float32(NEG))
        dist = q - k
        ok = dist >= 0
        biasT[:, 8 + h, 1, :] = np.where(ok, rel_bias[_t5_bucket_np(dist), 8 + h], np.float32(0))
        maskT[:, 8 + h, 1, :] = np.where(ok, np.float32(0), np.float32(NEG))
    return biasT, maskT


_NC_CACHE = {}


def kernel(x, mem, norm_mix_pre, norm_mix_post, norm_ffn_pre, norm_ffn_post, norm_mem,
           w_in, rel_bias, swa_sinks, w_mem_kv, w_branch_moba, w_branch_swa, w_branch_mem,
           w_out, w_ffn_up, ffn_conv_w, ffn_conv_b, w_ffn_down):
    f = lambda a: np.ascontiguousarray(np.asarray(a, dtype=np.float32))
    x = f(x)
    mem = f(mem)
    B, S, _ = x.shape
    NB = S // 256
    C0 = NB // 2 - 1
    NG = NB - C0
    half_tok = S // 2
    rel_bias = f(rel_bias)
    biasT, maskT = _bias_tiles(rel_bias)
    gpre = np.stack([f(norm_mix_pre)[0], f(norm_ffn_pre)[0], f(norm_mem)[0]], 0)
    gpre = np.ascontiguousarray(gpre.reshape(3, 8, 128).transpose(2, 0, 1))
    gpost = np.ascontiguousarray(np.stack([f(norm_mix_post)[0], f(norm_ffn_post)[0]], 0))
    cwv = np.concatenate([f(ffn_conv_w)[0], f(ffn_conv_b)[0][None, :]], 0)
    cwv = np.ascontiguousarray(cwv.reshape(4, 44, 128).transpose(2, 0, 1))
    common = {
        "w_in": f(w_in)[0], "w_mem_kv": f(w_mem_kv)[0],
        "w_br0": f(w_branch_moba)[0], "w_br1": f(w_branch_swa)[0], "w_br2": f(w_branch_mem)[0],
        "w_out": f(w_out)[0], "w_up": f(w_ffn_up)[0], "w_dn": f(w_ffn_down)[0],
        "gpre": gpre, "gpost": gpost, "cw": cwv, "biasT": biasT, "maskT": maskT,
        "rel_bias": rel_bias, "sinks": f(swa_sinks).reshape(1, 8),
    }
    in_maps = []
    for b in range(B):
        for half in range(2):
            if half == 1:
                xv = x[b]
            else:
                xv = np.concatenate([np.zeros((half_tok, D), np.float32), x[b, :half_tok]], 0)
            gm = np.full((NG, 32), -1e30, np.float32)
            for gi in range(NG):
                c = C0 + gi
                lo = 0 if half == 1 else NB // 2
                gm[gi, lo:c] = 0.0
            m = dict(common)
            m["xv"] = np.ascontiguousarray(xv)
            m["mem"] = mem[b]
            m["gmask"] = gm.reshape(1, NG * 32)
            m["firstmask"] = np.full((128, 128), 0.0 if half == 1 else NEG, np.float32)
            m["haloflag"] = np.full((128, 1), float(half), np.float32)
            in_maps.append(m)
    key = (NB, C0)
    if key not in _NC_CACHE:
        _NC_CACHE[key] = build(NB, C0)
    nc = _NC_CACHE[key]
    res = run_bass_kernel_spmd(nc, in_maps, core_ids=list(range(len(in_maps))))
    out = np.empty((B, S, D), np.float32)
    i = 0
    for b in range(B):
        for half in range(2):
            out[b, half * half_tok:(half + 1) * half_tok] = np.asarray(res.results[i]["out"], np.float32)
            i += 1
    return out
```

```python
import math
import os
import numpy as np
import ml_dtypes
import concourse.bass as bass
import concourse.mybir as mybir
from concourse.bass_utils import run_bass_kernel_spmd

F32 = mybir.dt.float32
BF16 = mybir.dt.bfloat16
AF = mybir.ActivationFunctionType
ALU = mybir.AluOpType
AX = mybir.AxisListType

D = 1024
FH = 2816
NEG = -30000.0


class Prog:
    ENGS = ['pe', 'act', 'dve', 'pool', 'sp']

    def __init__(self, nc):
        self.nc = nc
        self.stream = {e: [] for e in self.ENGS}
        self.semval = {}
        self.sems = {}
        self.waited = {}
        self.res = {}
        self.lastwait = {}
        self.nops = 0
        for e in self.ENGS:
            self._sem('c_' + e)

    def _sem(self, name):
        if name not in self.sems:
            self.sems[name] = self.nc.alloc_semaphore(name)
            self.semval[name] = 0
        return name

    def op(self, e, fn, reads=(), writes=(), dma=None):
        deps = {}
        self.nops += 1

        def add(s, v):
            if deps.get(s, 0) < v:
                deps[s] = v
        for r in reads:
            st = self.res.get(r)
            if st and st['w']:
                add(*st['w'])
        for w in writes:
            st = self.res.get(w)
            if st:
                if st['w']:
                    add(*st['w'])
                for s, v in st['r'].items():
                    add(s, v)
        if dma is None:
            sem, inc = 'c_' + e, 1
        else:
            sem, inc = self._sem('d_' + dma), 16
        waits = []
        for s, v in deps.items():
            if e == 'pe' and s == 'c_pe':
                continue
            if s.startswith('d_'):
                v = self.semval[s]
                self.lastwait[s] = max(self.lastwait.get(s, 0), v)
            if self.waited.get((e, s), 0) >= v:
                continue
            self.waited[(e, s)] = v
            waits.append((s, v))
        if dma is not None:
            lw = self.lastwait.get(sem, 0)
            if lw > self.waited.get((e, sem), 0):
                self.waited[(e, sem)] = lw
                waits.append((sem, lw))
        self.semval[sem] += inc
        tok = (sem, self.semval[sem])
        self.stream[e].append((waits, fn, sem, inc))
        for r in reads:
            st = self.res.setdefault(r, dict(w=None, r={}))
            st['r'][sem] = tok[1]
        for w in writes:
            self.res[w] = dict(w=tok, r={})
        return tok

    def wait_all(self, e, names):
        waits = [('d_' + s, self.semval['d_' + s]) for s in names if ('d_' + s) in self.semval]
        self.stream[e].append((waits, None, None, 0))

    def emit(self):
        nc = self.nc
        with nc.Block() as block:
            def mk(e):
                def body(eng):
                    for waits, fn, sem, inc in self.stream[e]:
                        for s, v in waits:
                            eng.wait_ge(self.sems[s], v)
                        if fn is not None:
                            fn(eng).then_inc(self.sems[sem], inc)
                return body
            block.tensor(mk('pe'))
            block.scalar(mk('act'))
            block.vector(mk('dve'))
            block.gpsimd(mk('pool'))
            block.sync(mk('sp'))


def build(NB, C0):
    NG = NB - C0
    NT = NB * 2
    nc = bass.Bass("TRN2", target_bir_lowering=False)

    def din(name, shape, dt=F32):
        return nc.dram_tensor(name, shape, dt, kind="ExternalInput").ap()
    xv = din("xv", [NB * 256, D])
    memx = din("mem", [256, D])
    w_in = din("w_in", [D, 5888])
    w_mkv = din("w_mem_kv", [D, 1024])
    w_br = [din("w_br%d" % i, [512, D]) for i in range(3)]
    w_out = din("w_out", [D, D])
    w_up = din("w_up", [D, 2 * FH])
    w_dn = din("w_dn", [FH, D])
    gpre_d = din("gpre", [128, 3, 8])
    gpost_d = din("gpost", [2, D])
    cw_d = din("cw", [128, 4, 44])
    biasT_d = din("biasT", [128, 16, 2, 128])
    maskT_d = din("maskT", [128, 16, 2, 128])
    relb_d = din("rel_bias", [32, 16])
    sinks_d = din("sinks", [1, 8])
    gmask_d = din("gmask", [1, NG * 32])
    fmask_d = din("firstmask", [128, 128])
    hflag_d = din("haloflag", [128, 1])
    outd = nc.dram_tensor("out", [(NB // 2) * 256, D], F32, kind="ExternalOutput").ap()
    win_b = nc.dram_tensor("win_b", [D, 5888], BF16).ap()
    wmkv_b = nc.dram_tensor("wmkv_b", [D, 1024], BF16).ap()
    wbr_b = [nc.dram_tensor("wbr_b%d" % i, [512, D], BF16).ap() for i in range(3)]
    wout_b = nc.dram_tensor("wout_b", [D, D], BF16).ap()
    wup_b = nc.dram_tensor("wup_b", [D, 2 * FH], BF16).ap()
    wdn_b = nc.dram_tensor("wdn_b", [FH, D], BF16).ap()
    vscr = nc.dram_tensor("vscr", [NT, 128, 520], BF16).ap()

    P = Prog(nc)
    STOP = int(os.environ.get('K_STOP', '0'))
    SKIP = os.environ.get('K_SKIP', '').split(',')

    class _Stop(Exception):
        pass

    def ckpt(k):
        if STOP == k:
            raise _Stop()
    sb = nc.alloc_sbuf_tensor
    KT = sb("KT", [128, 4, NB * 256], BF16)
    kmT = sb("kmT", [128, 4, 32], F32)
    kmTb = sb("kmTb", [128, 4, 32], BF16)
    KswT = sb("KswT", [128, 4, 128], BF16)
    Vsw = sb("Vsw", [128, 4, 2, 65], BF16)
    KmeT = sb("KmeT", [128, 4, 256], BF16)
    Vme = sb("Vme", [128, 2, 4, 129], BF16)
    bm = sb("bm", [128, 16, 2, 128], BF16)
    gpost = sb("gpost_s", [128, 2, D], F32)
    gpre = sb("gpre_s", [128, 3, 8], F32)
    cw = sb("cw_s", [128, 4, 44], F32)
    carry = sb("carry_s", [128, 44, 2], F32)
    ident = sb("ident", [128, 128], BF16)
    identf = sb("identf", [128, 128], F32)
    b31 = sb("b31", [128, 8], F32)
    expsink = sb("expsink", [128, 8], F32)
    gmask = sb("gmask_s", [128, NG, 32], F32)
    fmask = sb("fmask_s", [128, 128], F32)
    hflag = sb("hflag_s", [128, 1], F32)
    xt = sb("xt_s", [128, 2, D], F32)
    xs = sb("xs", [128, D], BF16)
    hT = sb("hT", [128, 8, 256], BF16)
    NSLAB = 3
    ws = [sb("ws%d" % i, [128, 4096], BF16) for i in range(NSLAB)]
    QTz = sb("QTz", [128, 8, 256], BF16)
    QswTz = sb("QswTz", [128, 8, 256], BF16)
    QmeT = sb("QmeT", [128, 4, 256], BF16)
    gs = [sb("gs%d" % i, [128, 256], F32) for i in range(2)]
    vown = sb("vown", [128, 2, 8, 65], BF16)
    vs = [sb("vs%d" % i, [128, 4, 8, 65], BF16) for i in range(2)]
    PT = [sb("PT%d" % i, [128, 512], BF16) for i in range(3)]
    tmp = [sb("tmp%d" % i, [128, 384], F32) for i in range(2)]
    acc = sb("acc", [128, 2, 8, 65], F32)
    gmt = sb("gmt", [128, 2, 8, 32], F32)
    top8 = sb("top8", [128, 8, 8], F32)
    thr = sb("thr", [128, 8], F32)
    msk = sb("msk", [128, 2, 8, 32], F32)
    rec = sb("rec", [128, 2, 8], F32)
    den = sb("den", [128, 2], F32)
    Otok = sb("Otok", [128, 2, 512], BF16)
    OT = sb("OT", [128, 4, 256], BF16)
    mergedf = sb("mergedf", [128, 8, 256], F32)
    tg = [sb("tg%d" % i, [128, 256], F32) for i in range(2)]
    mergedT = sb("mergedT", [128, 8, 256], BF16)
    ub = [sb("ub%d" % i, [128, 258], F32) for i in range(2)]
    tb = [sb("tb%d" % i, [128, 256], F32) for i in range(4)]
    gl = [sb("gl%d" % i, [128, 256], F32) for i in range(2)]
    actT = [sb("actT%d" % i, [128, 256], BF16) for i in range(4)]
    ss = sb("ss", [128, 4], F32)
    rr = sb("rr", [128, 2], F32)
    ps = nc.alloc_psum_tensor
    psA = [ps("psA%d" % i, [128, 512], F32) for i in range(2)]
    psS = [ps("psS%d" % i, [128, 512], F32) for i in range(2)]
    psO = [ps("psO%d" % i, [128, 512], F32) for i in range(2)]
    psT = ps("psT", [128, 1024], BF16)
    psG = ps("psG", [128, 512], F32)

    rot = {}

    def nxt(name, n):
        i = rot.get(name, 0)
        rot[name] = (i + 1) % n
        return i

    try:
        TAGS = {}
        def castw(dst, src, rows, tag):
            for r0 in range(0, rows, 128):
                P.op('pool', lambda e, r0=r0: e.dma_start(out=dst[r0:r0 + 128, :], in_=src[r0:r0 + 128, :]),
                     writes=['%s_%d' % (tag, r0), 'castchain'], dma='cast')
                TAGS.setdefault(tag, []).append('%s_%d' % (tag, r0))
        for r0 in range(0, D, 128):
            P.op('pool', lambda e, r0=r0: e.dma_start(out=win_b[r0:r0 + 128, 0:1536], in_=w_in[r0:r0 + 128, 0:1536]),
                 writes=['win_b'], dma='cast')
            P.op('pool', lambda e, r0=r0: e.dma_start(out=win_b[r0:r0 + 128, 2048:5888], in_=w_in[r0:r0 + 128, 2048:5888]),
                 writes=['win_b'], dma='cast')
            for two in range(2):
                P.op('pool', lambda e, r0=r0, two=two: e.dma_start(
                    out=win_b[r0:r0 + 128, 1536:2048].rearrange("r (j t c) -> r j t c", j=4, t=2)[:, :, two, :],
                    in_=w_in[r0:r0 + 128, 1536 + two * 256:1536 + (two + 1) * 256].rearrange("r (j c) -> r j c", j=4)),
                    writes=['win_b'], dma='cast')
        castw(wmkv_b, w_mkv, D, 'wmkv_b')
        for i in range(3):
            castw(wbr_b[i], w_br[i], 512, 'wbr_b%d' % i)
        castw(wout_b, w_out, D, 'wout_b')
        castw(wup_b, w_up, D, 'wup_b')
        castw(wdn_b, w_dn, FH, 'wdn_b')

        P.op('sp', lambda e: e.dma_start(out=gpre[:], in_=gpre_d[:, :, :]), writes=['gpre'], dma='c')
        P.op('sp', lambda e: e.dma_start(out=cw[:], in_=cw_d[:, :, :]), writes=['cw'], dma='c')
        for i in range(2):
            P.op('sp', lambda e, i=i: e.dma_start(out=gpost[:, i, :], in_=gpost_d[i:i + 1, :].broadcast_to([128, D])),
                 writes=['gpost'], dma='c')
        P.op('sp', lambda e: e.dma_start(out=b31[:], in_=relb_d[31:32, 0:8].broadcast_to([128, 8])), writes=['b31'], dma='c')
        P.op('sp', lambda e: e.dma_start(out=expsink[:], in_=sinks_d[0:1, :].broadcast_to([128, 8])), writes=['expsink'], dma='c')
        P.op('sp', lambda e: e.dma_start(out=gmask[:].rearrange("p g n -> p (g n)"), in_=gmask_d[0:1, :].broadcast_to([128, NG * 32])),
             writes=['gmask'], dma='c')
        P.op('sp', lambda e: e.dma_start(out=fmask[:], in_=fmask_d[:, :]), writes=['fmask'], dma='c')
        P.op('sp', lambda e: e.dma_start(out=hflag[:], in_=hflag_d[:, :]), writes=['hflag'], dma='c')
        P.op('act', lambda e: e.activation(out=expsink[:], in_=expsink[:], func=AF.Exp), reads=['expsink'], writes=['expsink'])
        xtf = xt[:].rearrange("p a b -> p (a b)")
        mff = mergedf[:].rearrange("p a b -> p (a b)")
        for hf in range(2):
            P.op('sp', lambda e, hf=hf: e.dma_start(out=xtf, in_=biasT_d[:, hf * 8:(hf + 1) * 8, :, :].rearrange("p a b c -> p (a b c)")),
                 writes=['xt0', 'xt1'], dma='c')
            P.op('sp', lambda e, hf=hf: e.dma_start(out=mff, in_=maskT_d[:, hf * 8:(hf + 1) * 8, :, :].rearrange("p a b c -> p (a b c)")),
                 writes=['mergedf'], dma='c')
            P.op('dve', lambda e, hf=hf: e.tensor_tensor(out=bm[:, hf * 8:(hf + 1) * 8, :, :].rearrange("p a b c -> p (a b c)"),
                                                         in0=xtf, in1=mff, op=ALU.add),
                 reads=['xt0', 'xt1', 'mergedf'], writes=['bm'])
        P.op('pool', lambda e: e.memset(identf[:], 0.0), writes=['identf'])
        P.op('pool', lambda e: e.affine_select(out=identf[:], in_=identf[:], pattern=[[-1, 128]], compare_op=ALU.not_equal,
                                               fill=1.0, base=0, channel_multiplier=1), reads=['identf'], writes=['identf'])
        P.op('dve', lambda e: e.tensor_copy(out=ident[:], in_=identf[:]), reads=['identf'], writes=['ident'])
        P.op('pool', lambda e: e.memset(kmT[:], 0.0), writes=['km'])
        P.op('pool', lambda e: e.memset(kmTb[:], 0.0), writes=['kmb'])
        P.op('pool', lambda e: e.memset(QTz[:], 0.0), writes=['QTz%d' % h for h in range(8)])
        P.op('pool', lambda e: e.memset(QswTz[:], 0.0), writes=['QswTz%d' % h for h in range(8)])
        P.op('pool', lambda e: e.memset(vown[:], 1.0), writes=['vown'])
        P.op('pool', lambda e: e.memset(Vsw[:], 1.0), writes=['Vsw%d' % i for i in range(4)])
        P.op('pool', lambda e: e.memset(Vme[:], 1.0), writes=['Vme'])
        P.op('pool', lambda e: e.memset(carry[:], 0.0), writes=['carry'])
        P.op('pool', lambda e: e.memset(KswT[:], 0.0), writes=['KswT%d' % i for i in range(4)])

        ckpt(1)
        def load_slab(parts):
            s = nxt('ws', NSLAB)
            for (src, nch, width, off, ncols, rd) in parts:
                dst = ws[s][:, 0:nch * width].rearrange("p (c n) -> p c n", c=nch)[:, :, off:off + ncols]
                P.op('sp', lambda e, dst=dst, src=src: e.dma_start(out=dst, in_=src.rearrange("(c p) n -> p c n", p=128)),
                     reads=TAGS.get(rd, [rd]), writes=['ws%d' % s], dma='ws%d' % s)
            return s

        def slabv(s, nch, width):
            return ws[s][:, 0:nch * width].rearrange("p (c n) -> p c n", c=nch)

        def norm_T(srcs, gi, ncols):
            for i, (src, rs) in enumerate(srcs):
                P.op('act', lambda e, src=src, i=i: e.activation(out=xs[:], in_=src, func=AF.Square, accum_out=ss[:, i:i + 1]),
                     reads=[rs], writes=['xs', 'ss'])
                P.op('act', lambda e, i=i: e.activation(out=rr[:, i:i + 1], in_=ss[:, i:i + 1], func=AF.Sqrt, scale=1.0 / D, bias=1e-6),
                     reads=['ss'], writes=['rr'])
                P.op('dve', lambda e, i=i: e.reciprocal(out=rr[:, i:i + 1], in_=rr[:, i:i + 1]), reads=['rr'], writes=['rr'])
                P.op('dve', lambda e, src=src, i=i: e.tensor_scalar(out=xs[:], in0=src, scalar1=rr[:, i:i + 1], scalar2=None, op0=ALU.mult),
                     reads=[rs, 'rr'], writes=['xs'])
                for c in range(8):
                    P.op('pe', lambda e, c=c: e.transpose(out=psT[:, c * 128:(c + 1) * 128], in_=xs[:, c * 128:(c + 1) * 128], identity=ident[:]),
                         reads=['xs', 'ident'], writes=['psT'])
                for c in range(8):
                    eng = 'act'
                    if eng == 'act':
                        P.op('act', lambda e, c=c, i=i: e.activation(out=hT[:, c, i * 128:(i + 1) * 128], in_=psT[:, c * 128:(c + 1) * 128],
                                                                     func=AF.Identity, scale=gpre[:, gi, c:c + 1]),
                             reads=['psT', 'gpre'], writes=['hT'])
                    else:
                        P.op('dve', lambda e, c=c, i=i: e.tensor_scalar(out=hT[:, c, i * 128:(i + 1) * 128], in0=psT[:, c * 128:(c + 1) * 128],
                                                                        scalar1=gpre[:, gi, c:c + 1], scalar2=None, op0=ALU.mult),
                             reads=['psT', 'gpre'], writes=['hT'])

        def fm_chunk(s, nch, width, col, ntok, rhs_t=None, rhs_res='hT'):
            a = nxt('psA', 2)
            sv = slabv(s, nch, width)
            src = hT if rhs_t is None else rhs_t
            for d in range(nch):
                P.op('pe', lambda e, d=d: e.matmul(psA[a][:, 0:ntok], lhsT=sv[:, d, col:col + 128], rhs=src[:, d, 0:ntok],
                                                   start=(d == 0), stop=(d == nch - 1)),
                     reads=['ws%d' % s, rhs_res], writes=['psA%d' % a])
            return a

        def tm_tile(s, nch, width, col, ncols, sub):
            a = nxt('psA', 2)
            sv = slabv(s, nch, width)
            for d in range(nch):
                P.op('pe', lambda e, d=d: e.matmul(psA[a][:, 0:ncols], lhsT=hT[:, d, sub * 128:(sub + 1) * 128], rhs=sv[:, d, col:col + ncols],
                                                   start=(d == 0), stop=(d == nch - 1)),
                     reads=['ws%d' % s, 'hT'], writes=['psA%d' % a])
            return a

        def load_x(n):
            for sub in range(2):
                P.op('sp', lambda e, sub=sub: e.dma_start(out=xt[:, sub, :], in_=xv[n * 256 + sub * 128:n * 256 + (sub + 1) * 128, :]),
                     writes=['xt%d' % sub], dma='x')

        def proj_kv(n):
            s = load_slab([(win_b[:, 512:1024], 8, 512, 0, 512, 'win_b')])
            for p in range(4):
                a = fm_chunk(s, 8, 512, p * 128, 256)
                P.op('act', lambda e, a=a, p=p: e.activation(out=KT[:, p, n * 256:(n + 1) * 256], in_=psA[a][:, 0:256], func=AF.Identity,
                                                             accum_out=kmT[:, p, n:n + 1]),
                     reads=['psA%d' % a], writes=['KT%d' % n, 'km'])
            if 'kmb' not in SKIP:
              P.op('dve', lambda e: e.tensor_copy(out=kmTb[:, :, n:n + 1], in_=kmT[:, :, n:n + 1]), reads=['km'], writes=['kmb'])
            s = load_slab([(win_b[:, 1024:1536], 8, 512, 0, 512, 'win_b')])
            for sub in range(2):
                a = tm_tile(s, 8, 512, 0, 512, sub)
                P.op('dve', lambda e, a=a, sub=sub: e.tensor_copy(out=vown[:, sub, :, 0:64], in_=psA[a][:, 0:512].rearrange("p (h c) -> p h c", h=8)),
                     reads=['psA%d' % a], writes=['vown'])
            for sub in range(2 if 'vst' not in SKIP else 0):
                P.op('pool', lambda e, sub=sub: e.dma_start(out=vscr[2 * n + sub, :, :], in_=vown[:, sub, :, :].rearrange("p h c -> p (h c)")),
                     reads=['vown'], writes=['vscr%d' % n], dma='vst')
            s = load_slab([(win_b[:, 2048:2304], 8, 256, 0, 256, 'win_b')])
            a = fm_chunk(s, 8, 256, 0, 256)
            for sub in range(2):
                sl = (2 * n + sub) % 4
                P.op('act', lambda e, a=a, sub=sub, sl=sl: e.copy(out=KswT[:, sl, :], in_=psA[a][:, sub * 128:(sub + 1) * 128]),
                     reads=['psA%d' % a], writes=['KswT%d' % sl])
            for sub in range(2):
                sl = (2 * n + sub) % 4
                a = tm_tile(s, 8, 256, 128, 128, sub)
                P.op('dve', lambda e, a=a, sl=sl: e.tensor_copy(out=Vsw[:, sl, :, 0:64], in_=psA[a][:, 0:128].rearrange("p (h c) -> p h c", h=2)),
                     reads=['psA%d' % a], writes=['Vsw%d' % sl])

        def otok_to_OT():
            for sub in range(2):
                for pc in range(4):
                    P.op('pe', lambda e, sub=sub, pc=pc: e.transpose(out=psT[:, pc * 128:(pc + 1) * 128], in_=Otok[:, sub, pc * 128:(pc + 1) * 128], identity=ident[:]),
                         reads=['Otok', 'ident'], writes=['psT'])
                P.op('act', lambda e, sub=sub: e.copy(out=OT[:, :, sub * 128:(sub + 1) * 128], in_=psT[:, 0:512].rearrange("p (c n) -> p c n", c=4)),
                     reads=['psT'], writes=['OT'])

        def branch_merge(b, pos):
            sb_ = load_slab([(wbr_b[b][:, :], 4, 1024, 0, 1024, 'wbr_b%d' % b)])
            bv = slabv(sb_, 4, 1024)
            for nh in range(2):
                sg = load_slab([(win_b[:, 2816 + b * 1024 + nh * 512:2816 + b * 1024 + (nh + 1) * 512], 8, 512, 0, 512, 'win_b')])
                for n4 in range(4):
                    ncx = nh * 4 + n4
                    a = fm_chunk(sg, 8, 512, n4 * 128, 256)
                    g = nxt('gs', 2)
                    P.op('act', lambda e, a=a, g=g: e.activation(out=gs[g][:], in_=psA[a][:, 0:256], func=AF.Sigmoid),
                         reads=['psA%d' % a], writes=['gs%d' % g])
                    hfx = nxt('psS', 2)
                    for hc in range(4):
                        P.op('pe', lambda e, hc=hc, hfx=hfx, ncx=ncx: e.matmul(psS[hfx][:, 0:256], lhsT=bv[:, hc, ncx * 128:(ncx + 1) * 128],
                                                                              rhs=OT[:, hc, :], start=(hc == 0), stop=(hc == 3)),
                             reads=['ws%d' % sb_, 'OT'], writes=['psS%d' % hfx])
                    if pos == 'first':
                        P.op('dve', lambda e, g=g, hfx=hfx, ncx=ncx: e.tensor_tensor(out=mergedf[:, ncx, :], in0=psS[hfx][:, 0:256], in1=gs[g][:], op=ALU.mult),
                             reads=['psS%d' % hfx, 'gs%d' % g], writes=['mergedf'])
                    else:
                        t = nxt('tg', 2)
                        P.op('dve', lambda e, g=g, hfx=hfx, t=t: e.tensor_tensor(out=tg[t][:], in0=psS[hfx][:, 0:256], in1=gs[g][:], op=ALU.mult),
                             reads=['psS%d' % hfx, 'gs%d' % g], writes=['tg%d' % t])
                        if pos == 'mid':
                            P.op('pool', lambda e, t=t, ncx=ncx: e.tensor_tensor(out=mergedf[:, ncx, :], in0=mergedf[:, ncx, :], in1=tg[t][:], op=ALU.add),
                                 reads=['tg%d' % t, 'mergedf'], writes=['mergedf'])
                        else:
                            P.op('pool', lambda e, t=t, ncx=ncx: e.tensor_tensor(out=mergedT[:, ncx, :], in0=mergedf[:, ncx, :], in1=tg[t][:], op=ALU.add),
                                 reads=['tg%d' % t, 'mergedf'], writes=['mergedT'])

        def post_norm_residual(pi, which, sub):
            pr = pi
            for nh in range(2):
                P.op('act', lambda e, nh=nh: e.activation(out=xs[:, nh * 512:(nh + 1) * 512], in_=pr[nh][0][:, 0:512], func=AF.Square,
                                                          accum_out=ss[:, 2 + nh:3 + nh]),
                     reads=[pr[nh][1]], writes=['xs', 'ss'])
            P.op('dve', lambda e: e.tensor_tensor(out=ss[:, 0:1], in0=ss[:, 2:3], in1=ss[:, 3:4], op=ALU.add), reads=['ss'], writes=['ss'])
            P.op('act', lambda e: e.activation(out=rr[:, 0:1], in_=ss[:, 0:1], func=AF.Sqrt, scale=1.0 / D, bias=1e-6), reads=['ss'], writes=['rr'])
            P.op('dve', lambda e: e.reciprocal(out=rr[:, 0:1], in_=rr[:, 0:1]), reads=['rr'], writes=['rr'])
            mv = mergedf[:].rearrange("p a b -> p (a b)")
            for nh in range(2):
                P.op('dve', lambda e, nh=nh: e.scalar_tensor_tensor(out=mv[:, sub * 1024 + nh * 512:sub * 1024 + (nh + 1) * 512], in0=pr[nh][0][:, 0:512],
                                                                   scalar=rr[:, 0:1], in1=gpost[:, which, nh * 512:(nh + 1) * 512],
                                                                   op0=ALU.mult, op1=ALU.mult),
                     reads=[pr[nh][1], 'rr', 'gpost'], writes=['mergedf'])
            P.op('pool', lambda e: e.tensor_tensor(out=xt[:, sub, :], in0=xt[:, sub, :], in1=mv[:, sub * 1024:(sub + 1) * 1024], op=ALU.add),
                 reads=['mergedf', 'xt%d' % sub], writes=['xt%d' % sub])

        for sub in range(2):
            P.op('sp', lambda e, sub=sub: e.dma_start(out=xt[:, sub, :], in_=memx[sub * 128:(sub + 1) * 128, :]),
                 writes=['xt%d' % sub], dma='x')
        norm_T([(xt[:, 0, :], 'xt0'), (xt[:, 1, :], 'xt1')], 2, 256)
        s = load_slab([(wmkv_b[:, 0:512], 8, 512, 0, 512, 'wmkv_b')])
        for h in range(4):
            a = fm_chunk(s, 8, 512, h * 128, 256)
            P.op('act', lambda e, a=a, h=h: e.copy(out=KmeT[:, h, :], in_=psA[a][:, 0:256]), reads=['psA%d' % a], writes=['KmeT'])
        s = load_slab([(wmkv_b[:, 512:1024], 8, 512, 0, 512, 'wmkv_b')])
        for sub in range(2):
            a = tm_tile(s, 8, 512, 0, 512, sub)
            P.op('dve', lambda e, a=a, sub=sub: e.tensor_copy(out=Vme[:, sub, :, 0:128], in_=psA[a][:, 0:512].rearrange("p (h c) -> p h c", h=4)),
                 reads=['psA%d' % a], writes=['Vme'])

        ckpt(2)
        for n in range(C0):
            load_x(n)
            norm_T([(xt[:, 0, :], 'xt0'), (xt[:, 1, :], 'xt1')], 0, 256)
            proj_kv(n)

        ckpt(3)
        SC = 0.125
        SCM = 1.0 / math.sqrt(128.0)
        def main_group(gi):
            c = C0 + gi
            load_x(c)
            norm_T([(xt[:, 0, :], 'xt0'), (xt[:, 1, :], 'xt1')], 0, 256)
            s = load_slab([(win_b[:, 0:512], 8, 512, 0, 512, 'win_b')])
            for p in range(4):
                a = fm_chunk(s, 8, 512, p * 128, 256)
                P.op('act', lambda e, a=a, p=p: e.copy(out=QTz[0:64, 2 * p, :], in_=psA[a][0:64, 0:256]),
                     reads=['psA%d' % a], writes=['QTz%d' % (2 * p)])
                P.op('dve', lambda e, a=a, p=p: e.tensor_copy(out=QTz[64:128, 2 * p + 1, :], in_=psA[a][64:128, 0:256]),
                     reads=['psA%d' % a], writes=['QTz%d' % (2 * p + 1)])
            proj_kv(c)
            s = load_slab([(win_b[:, 1536:2048], 8, 512, 0, 512, 'win_b')])
            for j in range(4):
                a = fm_chunk(s, 8, 512, j * 128, 256)
                P.op('act', lambda e, a=a, j=j: e.copy(out=QswTz[0:64, j, :], in_=psA[a][0:64, 0:256]),
                     reads=['psA%d' % a], writes=['QswTz%d' % j])
                P.op('dve', lambda e, a=a, j=j: e.tensor_copy(out=QswTz[64:128, j + 4, :], in_=psA[a][64:128, 0:256]),
                     reads=['psA%d' % a], writes=['QswTz%d' % (j + 4)])
            s = load_slab([(win_b[:, 2304:2816], 8, 512, 0, 512, 'win_b')])
            for h in range(4):
                a = fm_chunk(s, 8, 512, h * 128, 256)
                P.op('act', lambda e, a=a, h=h: e.copy(out=QmeT[:, h, :], in_=psA[a][:, 0:256]), reads=['psA%d' % a], writes=['QmeT'])

            ckpt(4)
            for h in range(4):
                si = nxt('psS', 2)
                for mt in range(2):
                    P.op('pe', lambda e, h=h, mt=mt, si=si: e.matmul(psS[si][:, mt * 256:(mt + 1) * 256], lhsT=KmeT[:, h, mt * 128:(mt + 1) * 128],
                                                                    rhs=QmeT[:, h, :], start=True, stop=True),
                         reads=['KmeT', 'QmeT'], writes=['psS%d' % si])
                pi = nxt('PT', 3)
                P.op('act', lambda e, si=si, pi=pi: e.activation(out=PT[pi][:], in_=psS[si][:], func=AF.Exp, scale=SCM),
                     reads=['psS%d' % si], writes=['PT%d' % pi])
                oi = nxt('psO', 2)
                for sub in range(2):
                    for mt in range(2):
                        P.op('pe', lambda e, h=h, mt=mt, sub=sub, pi=pi, oi=oi: e.matmul(
                            psO[oi][:, sub * 129:(sub + 1) * 129], lhsT=PT[pi][:, mt * 256 + sub * 128:mt * 256 + (sub + 1) * 128],
                            rhs=Vme[:, mt, h, :], start=(mt == 0), stop=(mt == 1)),
                            reads=['PT%d' % pi, 'Vme'], writes=['psO%d' % oi])
                ov = psO[oi][:, 0:258].rearrange("p (s c) -> p s c", s=2)
                P.op('dve', lambda e, ov=ov: e.reciprocal(out=den[:, 0:2], in_=ov[:, :, 128]), reads=['psO%d' % oi], writes=['den'])
                for sub in range(2):
                    P.op('dve', lambda e, ov=ov, sub=sub, h=h: e.tensor_scalar(out=Otok[:, sub, h * 128:(h + 1) * 128], in0=ov[:, sub, 0:128],
                                                                             scalar1=den[:, sub:sub + 1], scalar2=None, op0=ALU.mult),
                         reads=['psO%d' % oi, 'den'], writes=['Otok'])
            otok_to_OT()
            branch_merge(2, 'first')

            ckpt(5)
            for sub in range(2):
                t = 2 * c + sub
                slp, slo = (t - 1) % 4, t % 4
                for h in range(8):
                    g = h // 4
                    si = nxt('psS', 2)
                    for o, sl in enumerate((slp, slo)):
                        P.op('pe', lambda e, h=h, o=o, sl=sl, si=si, sub=sub: e.matmul(psS[si][:, o * 128:(o + 1) * 128], lhsT=KswT[:, sl, :],
                                                                                     rhs=QswTz[:, h, sub * 128:(sub + 1) * 128], start=True, stop=True),
                             reads=['KswT%d' % sl, 'QswTz%d' % h], writes=['psS%d' % si])
                    ti = nxt('tmp', 2)
                    P.op('dve', lambda e, si=si, ti=ti, h=h: e.scalar_tensor_tensor(out=tmp[ti][:, 0:256], in0=psS[si][:, 0:256], scalar=SC,
                                                                                   in1=bm[:, 8 + h, :, :].rearrange("p a b -> p (a b)"),
                                                                                   op0=ALU.mult, op1=ALU.add),
                         reads=['psS%d' % si, 'bm'], writes=['tmp%d' % ti])
                    if gi == 1 and sub == 0:
                        P.op('dve', lambda e, ti=ti: e.tensor_tensor(out=tmp[ti][:, 0:128], in0=tmp[ti][:, 0:128], in1=fmask[:], op=ALU.add),
                             reads=['tmp%d' % ti, 'fmask'], writes=['tmp%d' % ti])
                    pi = nxt('PT', 3)
                    P.op('act', lambda e, ti=ti, pi=pi: e.activation(out=PT[pi][:, 0:256], in_=tmp[ti][:, 0:256], func=AF.Exp),
                         reads=['tmp%d' % ti], writes=['PT%d' % pi])
                    oi = nxt('psO', 2)
                    for o, sl in enumerate((slp, slo)):
                        P.op('pe', lambda e, o=o, sl=sl, pi=pi, oi=oi, g=g: e.matmul(psO[oi][:, 0:65], lhsT=PT[pi][:, o * 128:(o + 1) * 128],
                                                                                   rhs=Vsw[:, sl, g, :], start=(o == 0), stop=(o == 1)),
                             reads=['PT%d' % pi, 'Vsw%d' % sl], writes=['psO%d' % oi])
                    P.op('dve', lambda e, oi=oi, h=h: e.tensor_scalar(out=den[:, 0:1], in0=psO[oi][:, 64:65], scalar1=expsink[:, h:h + 1], scalar2=None, op0=ALU.add),
                         reads=['psO%d' % oi, 'expsink'], writes=['den'])
                    P.op('dve', lambda e: e.reciprocal(out=den[:, 0:1], in_=den[:, 0:1]), reads=['den'], writes=['den'])
                    P.op('dve', lambda e, oi=oi, h=h, sub=sub: e.tensor_scalar(out=Otok[:, sub, h * 64:(h + 1) * 64], in0=psO[oi][:, 0:64],
                                                                              scalar1=den[:, 0:1], scalar2=None, op0=ALU.mult),
                         reads=['psO%d' % oi, 'den'], writes=['Otok'])
            otok_to_OT()
            branch_merge(1, 'mid')

            ckpt(6)
            for sub in range(2 if 'topk' not in SKIP else 0):
                for h in range(8):
                    P.op('pe', lambda e, h=h, sub=sub: e.matmul(psG[:, h * 32:(h + 1) * 32], lhsT=QTz[:, h, sub * 128:(sub + 1) * 128],
                                                               rhs=kmTb[:, h // 2, :], start=True, stop=True),
                         reads=['QTz%d' % h, 'kmb'], writes=['psG'])
                for h in range(8):
                    P.op('dve', lambda e, h=h, sub=sub: e.tensor_tensor(out=gmt[:, sub, h, :], in0=psG[:, h * 32:(h + 1) * 32], in1=gmask[:, gi, :], op=ALU.add),
                         reads=['psG', 'gmask'], writes=['gmt'])
                for h in range(8):
                    P.op('dve', lambda e, h=h, sub=sub: e.max(out=top8[:, h, :], in_=gmt[:, sub, h, :]), reads=['gmt'], writes=['top8'])
                P.op('dve', lambda e: e.tensor_scalar(out=thr[:], in0=top8[:, :, 2], scalar1=-1e29, scalar2=None, op0=ALU.max),
                     reads=['top8'], writes=['thr'])
                for h in range(8):
                    P.op('dve', lambda e, h=h, sub=sub: e.tensor_scalar(out=msk[:, sub, h, :], in0=gmt[:, sub, h, :], scalar1=thr[:, h:h + 1], scalar2=None, op0=ALU.is_ge),
                         reads=['gmt', 'thr'], writes=['msk'])
            for h in range(8):
                p = h // 2
                si = nxt('psS', 2)
                P.op('pe', lambda e, h=h, p=p, si=si: e.matmul(psS[si][:, 0:256], lhsT=KT[:, p, (2 * c) * 128:(2 * c + 1) * 128], rhs=QTz[:, h, :], start=True, stop=True),
                     reads=['KT%d' % c, 'QTz%d' % h], writes=['psS%d' % si])
                P.op('pe', lambda e, h=h, p=p, si=si: e.matmul(psS[si][:, 256:384], lhsT=KT[:, p, (2 * c + 1) * 128:(2 * c + 2) * 128], rhs=QTz[:, h, 128:256], start=True, stop=True),
                     reads=['KT%d' % c, 'QTz%d' % h], writes=['psS%d' % si])
                ti = nxt('tmp', 2)
                P.op('dve', lambda e, si=si, ti=ti, h=h: e.scalar_tensor_tensor(out=tmp[ti][:, 0:256], in0=psS[si][:, 0:256], scalar=SC,
                                                                               in1=bm[:, h, :, :].rearrange("p a b -> p (a b)"), op0=ALU.mult, op1=ALU.add),
                     reads=['psS%d' % si, 'bm'], writes=['tmp%d' % ti])
                P.op('dve', lambda e, si=si, ti=ti, h=h: e.scalar_tensor_tensor(out=tmp[ti][:, 256:384], in0=psS[si][:, 256:384], scalar=SC,
                                                                               in1=bm[:, h, 0, :], op0=ALU.mult, op1=ALU.add),
                     reads=['psS%d' % si, 'bm'], writes=['tmp%d' % ti])
                pi = nxt('PT', 3)
                P.op('act', lambda e, ti=ti, pi=pi: e.activation(out=PT[pi][:, 0:384], in_=tmp[ti][:, 0:384], func=AF.Exp),
                     reads=['tmp%d' % ti], writes=['PT%d' % pi])
                oi = nxt('psO', 2)
                P.op('pe', lambda e, pi=pi, oi=oi, h=h: e.matmul(psO[oi][:, 0:65], lhsT=PT[pi][:, 0:128], rhs=vown[:, 0, h, :], start=True, stop=True),
                     reads=['PT%d' % pi, 'vown'], writes=['psO%d' % oi])
                P.op('pe', lambda e, pi=pi, oi=oi, h=h: e.matmul(psO[oi][:, 65:130], lhsT=PT[pi][:, 128:256], rhs=vown[:, 0, h, :], start=True, stop=False),
                     reads=['PT%d' % pi, 'vown'], writes=['psO%d' % oi])
                P.op('pe', lambda e, pi=pi, oi=oi, h=h: e.matmul(psO[oi][:, 65:130], lhsT=PT[pi][:, 256:384], rhs=vown[:, 1, h, :], start=False, stop=True),
                     reads=['PT%d' % pi, 'vown'], writes=['psO%d' % oi])
                P.op('dve', lambda e, oi=oi, h=h: e.tensor_copy(out=acc[:, :, h, :], in_=psO[oi][:, 0:130].rearrange("p (s c) -> p s c", s=2)),
                     reads=['psO%d' % oi], writes=['acc%d' % h])
            def past_scores(h, n):
                p = h // 2
                si = nxt('psS', 2)
                for kt in range(2):
                    P.op('pe', lambda e, kt=kt: e.matmul(psS[si][:, kt * 256:(kt + 1) * 256], lhsT=KT[:, p, (2 * n + kt) * 128:(2 * n + kt + 1) * 128],
                                                        rhs=QTz[:, h, :], start=True, stop=True),
                         reads=['KT%d' % n, 'QTz%d' % h], writes=['psS%d' % si])
                pi = nxt('PT', 3)
                if n == c - 1:
                    ti = nxt('tmp', 2)
                    P.op('dve', lambda e: e.scalar_tensor_tensor(out=tmp[ti][:, 0:128], in0=psS[si][:, 256:384], scalar=SC,
                                                                 in1=bm[:, h, 1, :], op0=ALU.mult, op1=ALU.add),
                         reads=['psS%d' % si, 'bm'], writes=['tmp%d' % ti, 'psS%d' % si])
                    P.op('act', lambda e: e.activation(out=PT[pi][:, 0:256], in_=psS[si][:, 0:256], func=AF.Exp, scale=SC, bias=b31[:, h:h + 1]),
                         reads=['psS%d' % si, 'b31'], writes=['PT%d' % pi])
                    P.op('act', lambda e: e.activation(out=PT[pi][:, 384:512], in_=psS[si][:, 384:512], func=AF.Exp, scale=SC, bias=b31[:, h:h + 1]),
                         reads=['psS%d' % si, 'b31'], writes=['PT%d' % pi])
                    P.op('act', lambda e: e.activation(out=PT[pi][:, 256:384], in_=tmp[ti][:, 0:128], func=AF.Exp),
                         reads=['tmp%d' % ti], writes=['PT%d' % pi])
                else:
                    P.op('act', lambda e: e.activation(out=PT[pi][:], in_=psS[si][:], func=AF.Exp, scale=SC, bias=b31[:, h:h + 1]),
                         reads=['psS%d' % si, 'b31'], writes=['PT%d' % pi])
                return pi

            def past_pv(h, n, vi, s0, pi):
                oi = nxt('psO', 2)
                for sub in range(2):
                    for kt in range(2):
                        P.op('pe', lambda e, sub=sub, kt=kt: e.matmul(
                            psO[oi][:, sub * 65:(sub + 1) * 65], lhsT=PT[pi][:, kt * 256 + sub * 128:kt * 256 + (sub + 1) * 128],
                            rhs=vs[vi][:, 2 * (n - s0) + kt, h, :], start=(kt == 0), stop=(kt == 1)),
                            reads=['PT%d' % pi, 'vs%d' % vi], writes=['psO%d' % oi])
                for sub in range(2):
                    P.op('dve', lambda e, sub=sub: e.scalar_tensor_tensor(out=acc[:, sub, h, :], in0=psO[oi][:, sub * 65:(sub + 1) * 65],
                                                                         scalar=msk[:, sub, h, n:n + 1], in1=acc[:, sub, h, :],
                                                                         op0=ALU.mult, op1=ALU.add),
                         reads=['psO%d' % oi, 'msk', 'acc%d' % h], writes=['acc%d' % h])

            def load_vs(s0, s1):
                vi = nxt('vs', 2)
                P.op('pool', lambda e: e.dma_start(out=vs[vi][:, 0:2 * (s1 - s0), :, :].rearrange("p t h c -> p t (h c)"),
                                                   in_=vscr[2 * s0:2 * s1, :, :].rearrange("t p c -> p t c")),
                     reads=['vscr%d' % n for n in range(s0, s1)], writes=['vs%d' % vi], dma='vs%d' % vi)
                return vi

            pending = None
            for s0 in range(0, c, 2):
                s1 = min(s0 + 2, c)
                vi = load_vs(s0, s1)
                for h in range(8):
                    for n in range(s0, s1):
                        pi = past_scores(h, n)
                        if pending is not None:
                            past_pv(*pending)
                        pending = (h, n, vi, s0, pi)
            if pending is not None:
                past_pv(*pending)
            if 'mnorm' in SKIP:
                ckpt(7)
            P.op('dve', lambda e: e.reciprocal(out=rec[:], in_=acc[:, :, :, 64]), reads=['acc%d' % h for h in range(8)], writes=['rec'])
            for sub in range(2):
                P.op('dve', lambda e, sub=sub: e.tensor_tensor(out=Otok[:, sub, :].rearrange("p (h c) -> p h c", h=8), in0=acc[:, sub, :, 0:64],
                                                              in1=rec[:, sub, :].unsqueeze(2).broadcast_to([128, 8, 64]), op=ALU.mult),
                     reads=['acc%d' % h for h in range(8)] + ['rec'], writes=['Otok'])
            otok_to_OT()
            branch_merge(0, 'last')

            ckpt(7)
            so = [load_slab([(wout_b[:, nh * 512:(nh + 1) * 512], 8, 512, 0, 512, 'wout_b')]) for nh in range(2)]
            for sub in range(2):
                for nh in range(2):
                    sv = slabv(so[nh], 8, 512)
                    for d in range(8):
                        P.op('pe', lambda e, d=d, nh=nh, sub=sub, sv=sv: e.matmul(psA[nh][:, 0:512], lhsT=mergedT[:, d, sub * 128:(sub + 1) * 128], rhs=sv[:, d, :],
                                                                                start=(d == 0), stop=(d == 7)),
                             reads=['ws%d' % so[nh], 'mergedT'], writes=['psA%d' % nh])
                post_norm_residual([(psA[0], 'psA0'), (psA[1], 'psA1')], 0, sub)

            ckpt(8)
            norm_T([(xt[:, 0, :], 'xt0'), (xt[:, 1, :], 'xt1')], 1, 256)
            dacc = [[(psS[0], 'psS0'), (psS[1], 'psS1')], [(psO[0], 'psO0'), (psO[1], 'psO1')]]
            for j in range(11):
                s = load_slab([(wup_b[:, 256 * j:256 * (j + 1)], 8, 512, 0, 256, 'wup_b'),
                               (wup_b[:, FH + 256 * j:FH + 256 * (j + 1)], 8, 512, 256, 256, 'wup_b')])
                if gi > 0:
                    sd = load_slab([(wdn_b[256 * j:256 * (j + 1), :], 2, 1024, 0, 1024, 'wdn_b')])
                    dv = slabv(sd, 2, 1024)
                for cc in range(2):
                    tbi = []
                    for part in range(2):
                        ch = part * 22 + 2 * j + cc
                        a = fm_chunk(s, 8, 512, part * 256 + cc * 128, 256)
                        if gi == 0:
                            P.op('dve', lambda e, a=a, ch=ch: e.tensor_scalar(out=carry[:, ch, :], in0=psA[a][:, 254:256], scalar1=hflag[:, 0:1], scalar2=None, op0=ALU.mult),
                                 reads=['psA%d' % a, 'hflag'], writes=['carry'])
                            continue
                        u = nxt('ub', 2)
                        t = nxt('tb', 4)
                        tbi.append(t)
                        P.op('act', lambda e, a=a, u=u: e.copy(out=ub[u][:, 2:258], in_=psA[a][:, 0:256]), reads=['psA%d' % a], writes=['ub%d' % u])
                        P.op('pool', lambda e, u=u, ch=ch: e.tensor_copy(out=ub[u][:, 0:2], in_=carry[:, ch, :]), reads=['carry'], writes=['ub%d' % u])
                        P.op('act', lambda e, a=a, t=t, ch=ch: e.activation(out=tb[t][:], in_=psA[a][:, 0:256], func=AF.Identity,
                                                                            scale=cw[:, 2, ch:ch + 1], bias=cw[:, 3, ch:ch + 1]),
                             reads=['psA%d' % a, 'cw'], writes=['tb%d' % t])
                        P.op('dve', lambda e, u=u, t=t, ch=ch: e.scalar_tensor_tensor(out=tb[t][:], in0=ub[u][:, 1:257], scalar=cw[:, 1, ch:ch + 1], in1=tb[t][:],
                                                                                     op0=ALU.mult, op1=ALU.add),
                             reads=['ub%d' % u, 'tb%d' % t, 'cw'], writes=['tb%d' % t])
                        P.op('dve', lambda e, u=u, t=t, ch=ch: e.scalar_tensor_tensor(out=tb[t][:], in0=ub[u][:, 0:256], scalar=cw[:, 0, ch:ch + 1], in1=tb[t][:],
                                                                                     op0=ALU.mult, op1=ALU.add),
                             reads=['ub%d' % u, 'tb%d' % t, 'cw'], writes=['tb%d' % t])
                        P.op('pool', lambda e, u=u, ch=ch: e.tensor_copy(out=carry[:, ch, :], in_=ub[u][:, 256:258]), reads=['ub%d' % u], writes=['carry'])
                    if gi == 0:
                        continue
                    gli = nxt('gl', 2)
                    ai = nxt('actT', 4)
                    P.op('act', lambda e, gli=gli, t=tbi[0]: e.activation(out=gl[gli][:], in_=tb[t][:], func=AF.Gelu_apprx_tanh),
                         reads=['tb%d' % tbi[0]], writes=['gl%d' % gli])
                    P.op('dve', lambda e, gli=gli, ai=ai, t=tbi[1]: e.tensor_tensor(out=actT[ai][:], in0=gl[gli][:], in1=tb[t][:], op=ALU.mult),
                         reads=['gl%d' % gli, 'tb%d' % tbi[1]], writes=['actT%d' % ai])
                    fch = 2 * j + cc
                    for sub in range(2):
                        for nh in range(2):
                            P.op('pe', lambda e, ai=ai, sub=sub, nh=nh, cc=cc, dv=dv, fch=fch: e.matmul(
                                dacc[sub][nh][0][:, 0:512], lhsT=actT[ai][:, sub * 128:(sub + 1) * 128], rhs=dv[:, cc, nh * 512:(nh + 1) * 512],
                                start=(fch == 0), stop=(fch == 21)),
                                reads=['actT%d' % ai, 'ws%d' % sd], writes=[dacc[sub][nh][1]])
            if gi == 0:
                return
            for sub in range(2):
                post_norm_residual(dacc[sub], 1, sub)
                r0 = (c - C0 - 1) * 256 + sub * 128
                P.op('pool', lambda e, sub=sub, r0=r0: e.dma_start(out=outd[r0:r0 + 128, :], in_=xt[:, sub, :]),
                     reads=['xt%d' % sub], dma='out')
        for gi in range(NG):
            main_group(gi)
    except _Stop:
        pass
    P.wait_all('pool', ['out', 'cast', 'vst'])
    P.wait_all('sp', ['c', 'x', 'ws0', 'ws1', 'ws2', 'vs0', 'vs1'])
    P.emit()
    return nc


def _t5_bucket_np(dist):
    n = np.maximum(dist, 0)
    nf = np.maximum(n, 1).astype(np.float32)
    large = 16 + (np.log(nf / np.float32(16)) / np.float32(math.log(128 / 16)) * np.float32(16)).astype(np.int32)
    large = np.minimum(large, 31)
    return np.where(n < 16, n, large)


def _bias_tiles(rel_bias):
    k = np.arange(128)[:, None]
    q = np.arange(128)[None, :]
    biasT = np.zeros((128, 16, 2, 128), np.float32)
    maskT = np.zeros((128, 16, 2, 128), np.float32)
    for h in range(8):
        for o in range(2):
            dist = o * 128 + q - k
            ok = dist >= 0
            biasT[:, h, o, :] = np.where(ok, rel_bias[_t5_bucket_np(dist), h], np.float32(0))
            maskT[:, h, o, :] = np.where(ok, np.float32(0), np.float32(NEG))
        dist = 128 + q - k
        ok = dist < 128
        biasT[:, 8 + h, 0, :] = np.where(ok, rel_bias[_t5_bucket_np(dist), 8 + h], np.float32(0))
        maskT[:, 8 + h, 0, :] = np.where(ok, np.float32(0), np.float32(NEG))
        dist = q - k
        ok = dist >= 0
        biasT[:, 8 + h, 1, :] = np.where(ok, rel_bias[_t5_bucket_np(dist), 8 + h], np.float32(0))
        maskT[:, 8 + h, 1, :] = np.where(ok, np.float32(0), np.float32(NEG))
    return biasT, maskT


_NC_CACHE = {}


def kernel(x, mem, norm_mix_pre, norm_mix_post, norm_ffn_pre, norm_ffn_post, norm_mem,
           w_in, rel_bias, swa_sinks, w_mem_kv, w_branch_moba, w_branch_swa, w_branch_mem,
           w_out, w_ffn_up, ffn_conv_w, ffn_conv_b, w_ffn_down):
    f = lambda a: np.ascontiguousarray(np.asarray(a, dtype=np.float32))
    x = f(x)
    mem = f(mem)
    B, S, _ = x.shape
    NB = S // 256
    C0 = NB // 2 - 1
    NG = NB - C0
    half_tok = S // 2
    rel_bias = f(rel_bias)
    biasT, maskT = _bias_tiles(rel_bias)
    gpre = np.stack([f(norm_mix_pre)[0], f(norm_ffn_pre)[0], f(norm_mem)[0]], 0)
    gpre = np.ascontiguousarray(gpre.reshape(3, 8, 128).transpose(2, 0, 1))
    gpost = np.ascontiguousarray(np.stack([f(norm_mix_post)[0], f(norm_ffn_post)[0]], 0))
    cwv = np.concatenate([f(ffn_conv_w)[0], f(ffn_conv_b)[0][None, :]], 0)
    cwv = np.ascontiguousarray(cwv.reshape(4, 44, 128).transpose(2, 0, 1))
    common = {
        "w_in": f(w_in)[0], "w_mem_kv": f(w_mem_kv)[0],
        "w_br0": f(w_branch_moba)[0], "w_br1": f(w_branch_swa)[0], "w_br2": f(w_branch_mem)[0],
        "w_out": f(w_out)[0], "w_up": f(w_ffn_up)[0], "w_dn": f(w_ffn_down)[0],
        "gpre": gpre, "gpost": gpost, "cw": cwv, "biasT": biasT, "maskT": maskT,
        "rel_bias": rel_bias, "sinks": f(swa_sinks).reshape(1, 8),
    }
    in_maps = []
    for b in range(B):
        for half in range(2):
            if half == 1:
                xv = x[b]
            else:
                xv = np.concatenate([np.zeros((half_tok, D), np.float32), x[b, :half_tok]], 0)
            gm = np.full((NG, 32), -1e30, np.float32)
            for gi in range(NG):
                c = C0 + gi
                lo = 0 if half == 1 else NB // 2
                gm[gi, lo:c] = 0.0
            m = dict(common)
            m["xv"] = np.ascontiguousarray(xv)
            m["mem"] = mem[b]
            m["gmask"] = gm.reshape(1, NG * 32)
            m["firstmask"] = np.full((128, 128), 0.0 if half == 1 else NEG, np.float32)
            m["haloflag"] = np.full((128, 1), float(half), np.float32)
            in_maps.append(m)
    key = (NB, C0)
    if key not in _NC_CACHE:
        _NC_CACHE[key] = build(NB, C0)
    nc = _NC_CACHE[key]
    res = run_bass_kernel_spmd(nc, in_maps, core_ids=list(range(len(in_maps))))
    out = np.empty((B, S, D), np.float32)
    i = 0
    for b in range(B):
        for half in range(2):
            out[b, half * half_tok:(half + 1) * half_tok] = np.asarray(res.results[i]["out"], np.float32)
            i += 1
    return out
```
